# Optimizing a Trainium2 kernel written in Bass

```python
import math
import jax, jax.numpy as jnp
from jax import lax
import numpy as np

D_MODEL = 1024
BATCH = 16
SEQ = 2048
DEPTH = 4

GRID_W = 64
CTX_LEN = 256
ATTN_HEADS = 4
HEAD_DIM = 64
ATTN_W = ATTN_HEADS * 2 * HEAD_DIM
ATTN_SCALE = HEAD_DIM ** -0.5
RG_W = D_MODEL - ATTN_W
RG_BLOCKS = 8
RG_BW = RG_W // RG_BLOCKS
RG_C = 8.0
CONV_W = 4
CONV_PAD_L = CONV_W // 2
CONV_PAD_R = CONV_W - 1 - CONV_PAD_L
CTX_COLS = 2 * ATTN_W + RG_W
IN_W = CTX_COLS + ATTN_W + RG_W
D_FF = 2816
N_EXPERTS = 8
TOP_K = 2
Q_BLOCK = 128
ROPE_BASE = 10000.0
ROPE_PAIRS = HEAD_DIM // 4
ALPHA = (2 * DEPTH) ** 0.25
BETA = (8 * DEPTH) ** -0.25
LN_EPS = 1e-5
RMS_EPS = 1e-5

kernel_name = 'hybrid_diffattn_rglru_moe_dit'

F32 = jnp.float32


def layer_norm(x, g, b):
    xf = x.astype(F32)
    mu = jnp.mean(xf, -1, keepdims=True)
    var = jnp.mean(jnp.square(xf - mu), -1, keepdims=True)
    return ((xf - mu) * lax.rsqrt(var + LN_EPS) * g + b).astype(x.dtype)


def adaln(cvec, w, b):
    m = (jax.nn.silu(cvec.astype(F32)) @ w + b).astype(cvec.dtype)
    return [t[:, None, :] for t in jnp.split(m, 6, axis=-1)]


def modulate(h, shift, scale):
    return h * (1 + scale) + shift


def axial_rope_tables(rows):
    row = jnp.repeat(jnp.arange(rows, dtype=F32), GRID_W)
    col = jnp.tile(jnp.arange(GRID_W, dtype=F32), rows)
    inv_freq = ROPE_BASE ** (-jnp.arange(ROPE_PAIRS, dtype=F32) / ROPE_PAIRS)
    ang = jnp.concatenate([row[:, None] * inv_freq, col[:, None] * inv_freq], -1)
    return jnp.cos(ang), jnp.sin(ang)


def apply_rope(t, cos, sin):
    tp = t.astype(F32).reshape(*t.shape[:-1], HEAD_DIM // 2, 2)
    cs = cos[None, :, None, None, :]
    sn = sin[None, :, None, None, :]
    te, to = tp[..., 0], tp[..., 1]
    return jnp.stack([te * cs - to * sn, te * sn + to * cs], -1).reshape(t.shape)


def qk_heads(t):
    return t.reshape(*t.shape[:2], ATTN_HEADS, 2, HEAD_DIM)


def v_heads(t):
    return t.reshape(*t.shape[:2], ATTN_HEADS, 2 * HEAD_DIM)


def diff_attn(q, k, v, lam):
    s = jnp.einsum('bqhmd,bkhmd->bhmqk', q, k, preferred_element_type=F32) * ATTN_SCALE
    p = jax.nn.softmax(s, axis=-1)
    a = p[:, :, 0] - lam * p[:, :, 1]
    return jnp.einsum('bhqk,bkhe->bqhe', a, v.astype(F32))


def diff_attn_blocked(q, k, v, lam):
    b, l = q.shape[:2]
    nb = l // Q_BLOCK
    qb = jnp.moveaxis(q.reshape(b, nb, Q_BLOCK, *q.shape[2:]), 1, 0)
    ob = lax.map(lambda qi: diff_attn(qi, k, v, lam), qb)
    return jnp.moveaxis(ob, 0, 1).reshape(b, l, *ob.shape[3:])


def diff_attn_finish(o, g, lam_init, dtype):
    o = o * lax.rsqrt(jnp.mean(o * o, -1, keepdims=True) + RMS_EPS) * g * (1.0 - lam_init)
    return o.reshape(*o.shape[:2], ATTN_W).astype(dtype)


def short_conv(x, w, b):
    l = x.shape[1]
    xp = jnp.pad(x, ((0, 0), (CONV_PAD_L, CONV_PAD_R), (0, 0)))
    y = b + xp[:, 0:l] * w[0]
    for j in range(1, CONV_W):
        y = y + xp[:, j:j + l] * w[j]
    return y


def rglru_coeffs(xc, wa, ba, wx, bx, lam):
    b, l, _ = xc.shape
    xf = xc.astype(F32)
    xb = xf.reshape(b, l, RG_BLOCKS, RG_BW)
    r = jax.nn.sigmoid(jnp.einsum('blgi,gij->blgj', xb, wa).reshape(b, l, RG_W) + ba)
    gi = jax.nn.sigmoid(jnp.einsum('blgi,gij->blgj', xb, wx).reshape(b, l, RG_W) + bx)
    log_a = -RG_C * r * jax.nn.softplus(-lam.astype(F32))
    return jnp.exp(log_a), jnp.sqrt(-jnp.expm1(2.0 * log_a)) * (gi * xf)


def _scan_combine(lhs, rhs):
    return (lhs[0] * rhs[0], rhs[0] * lhs[1] + rhs[1])


def linear_scan(a, bt, h0):
    acum, hcum = lax.associative_scan(_scan_combine, (a, bt), axis=1)
    return hcum + acum * h0[:, None, :]


def bidir_rglru(xr_ctx, xr_lat, conv_w, conv_b, wa, ba, wx, bx, lam, want_ctx):
    xc = short_conv(xr_ctx, conv_w, conv_b)
    xl = short_conv(xr_lat, conv_w, conv_b)
    h0 = jnp.zeros((xc.shape[0], RG_W), F32)
    out_c, out_l = None, 0.0
    for d in range(2):
        a_c, b_c = rglru_coeffs(xc, wa[d], ba[d], wx[d], bx[d], lam[d])
        a_l, b_l = rglru_coeffs(xl, wa[d], ba[d], wx[d], bx[d], lam[d])
        if d == 1:
            a_c, b_c, a_l, b_l = [jnp.flip(t, 1) for t in (a_c, b_c, a_l, b_l)]
        h_c = linear_scan(a_c, b_c, h0)
        h_l = linear_scan(a_l, b_l, h_c[:, -1])
        if d == 1:
            h_c, h_l = jnp.flip(h_c, 1), jnp.flip(h_l, 1)
        out_l = out_l + h_l
        if want_ctx:
            out_c = h_c if out_c is None else out_c + h_c
    return out_c, out_l


def swiglu(u, w1, w3, w2):
    return (jax.nn.silu(u @ w1) * (u @ w3)) @ w2


def moe_swiglu(u, router, w1, w3, w2):
    logits = (u @ router).astype(F32)
    top_v, top_i = lax.top_k(logits, TOP_K)
    gates = jax.nn.softmax(top_v, axis=-1)
    combine = jnp.sum(jax.nn.one_hot(top_i, N_EXPERTS, dtype=F32) * gates[..., None], axis=-2)
    out = jnp.zeros(u.shape, F32)
    for e in range(N_EXPERTS):
        out = out + combine[..., e:e + 1] * swiglu(u, w1[e], w3[e], w2[e]).astype(F32)
    return out.astype(u.dtype)


def setup_inputs(seed: int = 0) -> dict:
    key = jax.random.key(seed)
    ks = iter(jax.random.split(key, 40))
    nrm = lambda shape, s: jax.random.normal(next(ks), shape, F32) * s
    n_dense = (DEPTH + 1) // 2
    n_moe = DEPTH // 2
    u = jax.random.uniform(next(ks), (DEPTH, 2, RG_W), F32, minval=0.9, maxval=0.999)
    a0 = u ** (1.0 / RG_C)
    rg_lambda = jnp.log(a0) - jnp.log1p(-a0)
    return {
        'x': nrm((BATCH, SEQ, D_MODEL), 1.0),
        'c': nrm((BATCH, D_MODEL), 1.0),
        'ctx': nrm((BATCH, CTX_LEN, D_MODEL), 1.0),
        'c_ctx': nrm((D_MODEL,), 1.0),
        'w_mod': nrm((DEPTH, D_MODEL, 6 * D_MODEL), 0.5 * D_MODEL ** -0.5),
        'b_mod': nrm((DEPTH, 6 * D_MODEL), 0.01),
        'w_in': nrm((DEPTH, D_MODEL, IN_W), D_MODEL ** -0.5),
        'lam_q1': nrm((DEPTH, HEAD_DIM), 0.1),
        'lam_k1': nrm((DEPTH, HEAD_DIM), 0.1),
        'lam_q2': nrm((DEPTH, HEAD_DIM), 0.1),
        'lam_k2': nrm((DEPTH, HEAD_DIM), 0.1),
        'subln_g': 1.0 + nrm((DEPTH, 2 * HEAD_DIM), 0.01),
        'conv_w': nrm((DEPTH, CONV_W, RG_W), CONV_W ** -0.5),
        'conv_b': nrm((DEPTH, RG_W), 0.01),
        'rg_wa': nrm((DEPTH, 2, RG_BLOCKS, RG_BW, RG_BW), RG_BW ** -0.5),
        'rg_ba': nrm((DEPTH, 2, RG_W), 0.01),
        'rg_wx': nrm((DEPTH, 2, RG_BLOCKS, RG_BW, RG_BW), RG_BW ** -0.5),
        'rg_bx': nrm((DEPTH, 2, RG_W), 0.01),
        'rg_lambda': rg_lambda,
        'w_out': nrm((DEPTH, D_MODEL, D_MODEL), BETA * D_MODEL ** -0.5),
        'ln1_g': 1.0 + nrm((DEPTH, D_MODEL), 0.01),
        'ln1_b': nrm((DEPTH, D_MODEL), 0.01),
        'ln2_g': 1.0 + nrm((DEPTH, D_MODEL), 0.01),
        'ln2_b': nrm((DEPTH, D_MODEL), 0.01),
        'ffn_w1': nrm((n_dense, D_MODEL, D_FF), D_MODEL ** -0.5),
        'ffn_w3': nrm((n_dense, D_MODEL, D_FF), D_MODEL ** -0.5),
        'ffn_w2': nrm((n_dense, D_FF, D_MODEL), BETA * D_FF ** -0.5),
        'moe_router': nrm((n_moe, D_MODEL, N_EXPERTS), D_MODEL ** -0.5),
        'moe_w1': nrm((n_moe, N_EXPERTS, D_MODEL, D_FF), D_MODEL ** -0.5),
        'moe_w3': nrm((n_moe, N_EXPERTS, D_MODEL, D_FF), D_MODEL ** -0.5),
        'moe_w2': nrm((n_moe, N_EXPERTS, D_FF, D_MODEL), BETA * D_FF ** -0.5),
    }


def reference(x, c, ctx, c_ctx, w_mod, b_mod, w_in, lam_q1, lam_k1, lam_q2, lam_k2, subln_g,
              conv_w, conv_b, rg_wa, rg_ba, rg_wx, rg_bx, rg_lambda, w_out,
              ln1_g, ln1_b, ln2_g, ln2_b, ffn_w1, ffn_w3, ffn_w2,
              moe_router, moe_w1, moe_w3, moe_w2):
    dt = x.dtype
    rows = x.shape[1] // GRID_W
    cos, sin = axial_rope_tables(rows)
    h_lat, h_ctx = x, ctx

    def channel_mix(u, i):
        if i % 2 == 0:
            j = i // 2
            return swiglu(u, ffn_w1[j], ffn_w3[j], ffn_w2[j])
        j = i // 2
        return moe_swiglu(u, moe_router[j], moe_w1[j], moe_w3[j], moe_w2[j])

    for i in range(DEPTH):
        last = i == DEPTH - 1
        sh1, sc1, g1, sh2, sc2, g2 = adaln(c, w_mod[i], b_mod[i])
        csh1, csc1, cg1, csh2, csc2, cg2 = adaln(c_ctx[None], w_mod[i], b_mod[i])
        lam_init = 0.8 - 0.6 * math.exp(-0.3 * i)
        lam = (jnp.exp(jnp.sum(lam_q1[i] * lam_k1[i]).astype(F32))
               - jnp.exp(jnp.sum(lam_q2[i] * lam_k2[i]).astype(F32)) + lam_init)

        p_lat = modulate(h_lat, sh1, sc1) @ w_in[i]
        p_ctx = modulate(h_ctx, csh1, csc1) @ (w_in[i][:, :CTX_COLS] if last else w_in[i])
        k_l = apply_rope(qk_heads(p_lat[..., :ATTN_W]), cos, sin)
        v_l = v_heads(p_lat[..., ATTN_W:2 * ATTN_W])
        xr_l = p_lat[..., 2 * ATTN_W:CTX_COLS]
        q_l = apply_rope(qk_heads(p_lat[..., CTX_COLS:CTX_COLS + ATTN_W]), cos, sin)
        y_l = p_lat[..., CTX_COLS + ATTN_W:]
        k_c = qk_heads(p_ctx[..., :ATTN_W]).astype(F32)
        v_c = v_heads(p_ctx[..., ATTN_W:2 * ATTN_W])
        xr_c = p_ctx[..., 2 * ATTN_W:CTX_COLS]

        k_all = jnp.concatenate([k_c, k_l], axis=1)
        v_all = jnp.concatenate([v_c, v_l], axis=1)
        att_l = diff_attn_finish(diff_attn_blocked(q_l, k_all, v_all, lam), subln_g[i], lam_init, dt)

        rg_c, rg_lat = bidir_rglru(xr_c, xr_l, conv_w[i], conv_b[i], rg_wa[i], rg_ba[i],
                                   rg_wx[i], rg_bx[i], rg_lambda[i], not last)
        rg_l = (rg_lat * jax.nn.gelu(y_l.astype(F32))).astype(dt)

        mix_l = jnp.concatenate([att_l, rg_l], axis=-1) @ w_out[i]
        h_lat = layer_norm(ALPHA * h_lat + (1 + g1) * mix_l, ln1_g[i], ln1_b[i])
        h_lat = layer_norm(ALPHA * h_lat + (1 + g2) * channel_mix(modulate(h_lat, sh2, sc2), i),
                           ln2_g[i], ln2_b[i])

        if not last:
            q_c = qk_heads(p_ctx[..., CTX_COLS:CTX_COLS + ATTN_W]).astype(F32)
            y_c = p_ctx[..., CTX_COLS + ATTN_W:]
            att_c = diff_attn_finish(diff_attn(q_c, k_c, v_c, lam), subln_g[i], lam_init, dt)
            rgc = (rg_c * jax.nn.gelu(y_c.astype(F32))).astype(dt)
            mix_c = jnp.concatenate([att_c, rgc], axis=-1) @ w_out[i]
            h_ctx = layer_norm(ALPHA * h_ctx + (1 + cg1) * mix_c, ln1_g[i], ln1_b[i])
            h_ctx = layer_norm(ALPHA * h_ctx + (1 + cg2) * channel_mix(modulate(h_ctx, csh2, csc2), i),
                               ln2_g[i], ln2_b[i])
    return h_lat
```

```python
import math
from contextlib import ExitStack

import numpy as np
import ml_dtypes

import concourse.bass as bass
import concourse.mybir as mybir
from concourse.bass_utils import run_bass_kernel_spmd

F32 = mybir.dt.float32
BF16 = mybir.dt.bfloat16
ALU = mybir.AluOpType
AF = mybir.ActivationFunctionType
AX = mybir.AxisListType

D = 1024
DEPTH = 4
SEQ = 2048
CTX = 256
T = SEQ + CTX
IN_W = 2560
D_FF = 2816
NE = 8
ALPHA = (2 * DEPTH) ** 0.25
LN_EPS = 1e-5 / (ALPHA * ALPHA)
RMS_EPS = 1e-5
ATTN_SCALE = 64 ** -0.5
TILES = [(0, 256), (256, 512), (768, 512), (1280, 512), (1792, 512)]
NFG = D_FF // 256

CFG = {"NL": 4, "NSEQ": 2, "NCORES": 8, "STOP": "ln2"}
_ORDER = ["load", "rg", "attn", "ln1", "ffn", "ln2"]


def run(stage):
    return _ORDER.index(stage) <= _ORDER.index(CFG["STOP"])


class Sync:
    def __init__(self, nc, es):
        self.nc = nc
        self.eng = {"pe": nc.tensor, "act": nc.scalar, "dve": nc.vector, "pool": nc.gpsimd, "sp": nc.sync}
        self.sem, self.cnt, self.seen, self.state = {}, {}, {}, {}
        self.es = es
        for e in self.eng:
            self.newsem(e)

    def newsem(self, name):
        self.sem[name] = self.es.enter_context(self.nc.semaphore(name))
        self.cnt[name] = 0

    def _deps(self, me, reads, writes):
        deps = {}

        def add(p):
            if p is not None and deps.get(p[0], 0) < p[1]:
                deps[p[0]] = p[1]
        for k in reads:
            st = self.state.get(k)
            if st:
                add(st[0])
                if isinstance(k, tuple) and k[0] == "ps":
                    for s, v in st[1].items():
                        if s != me:
                            add((s, v))
        for k in writes:
            st = self.state.get(k)
            if st:
                add(st[0])
                for s, v in st[1].items():
                    add((s, v))
        return deps

    def _wait(self, me, deps):
        seen = self.seen.setdefault(me, {})
        for s, v in deps.items():
            if s == me and me == "pe":
                continue
            if seen.get(s, 0) >= v:
                continue
            self.eng[me].wait_ge(self.sem[s], v)
            seen[s] = v

    def _record(self, tag, reads, writes):
        for k in reads:
            st = self.state.setdefault(k, [None, {}])
            st[1][tag[0]] = tag[1]
        for k in writes:
            self.state[k] = [tag, {}]

    def op(self, me, fn, reads=(), writes=()):
        self._wait(me, self._deps(me, reads, writes))
        fn(self.eng[me]).then_inc(self.sem[me], 1)
        self.cnt[me] += 1
        self._record((me, self.cnt[me]), reads, writes)

    def dma(self, queue, semname, xfers, reads=(), writes=()):
        if semname not in self.sem:
            self.newsem(semname)
        self._wait(queue, self._deps(semname, reads, writes))
        for (o, i) in xfers:
            self.eng[queue].dma_start(out=o, in_=i).then_inc(self.sem[semname], 16)
            self.cnt[semname] += 16
        self._record((semname, self.cnt[semname]), reads, writes)

    def barrier(self):
        for me in self.eng:
            self._wait(me, {s: v for s, v in self.cnt.items() if v > 0 and s != me})

    def final_wait(self, me, semname):
        self._wait(me, {semname: self.cnt[semname]})


def build(NL, NSEQ):
    nc = bass.Bass("TRN2", target_bir_lowering=False)

    def din(name, shape, dt=F32):
        return nc.dram_tensor(name, list(shape), dt, kind="ExternalInput").ap()

    xT = din("xT", [NSEQ, D, SEQ])
    ctxT = din("ctxT", [NSEQ, D, CTX])
    cT = din("cT", [128, 8, 3])
    w_mod = din("w_mod", [DEPTH, D, 6 * D])
    b_modT = din("b_modT", [128, DEPTH, 48])
    w_in = din("w_in", [DEPTH, D, IN_W])
    w_out = din("w_out", [DEPTH, D, D])
    lamqk = din("lamqk", [128, DEPTH, 4, 64])
    sublnT = din("sublnT", [128, DEPTH])
    convT = din("convT", [128, DEPTH, 4, 5])
    rgvT = din("rgvT", [128, DEPTH, 2, 4, 3])
    gate_bd = din("gate_bd", [DEPTH, 2, 4, 2, 128, 128])
    lnT = din("lnT", [128, DEPTH, 4, 8])
    ffn_w1 = din("ffn_w1", [2, D, D_FF])
    ffn_w3 = din("ffn_w3", [2, D, D_FF])
    ffn_w2 = din("ffn_w2", [2, D_FF, D])
    routerT = din("routerT", [128, 2, 8, NE])
    moe_w1 = din("moe_w1", [2, NE, D, D_FF])
    moe_w3 = din("moe_w3", [2, NE, D, D_FF])
    moe_w2 = din("moe_w2", [2, NE, D_FF, D])
    cosd = din("cosd", [128, SEQ])
    sind = din("sind", [128, SEQ])
    constf = din("constf", [128, 4, 128])
    constb = din("constb", [128, 2, 128], BF16)
    yT = nc.dram_tensor("yT", [NSEQ, D, SEQ], F32, kind="ExternalOutput").ap()

    es = ExitStack()
    with es:
        S = Sync(nc, es)

        uid = [0]

        def sb(scope, name, shape, dt=F32):
            uid[0] += 1
            return scope.enter_context(nc.sbuf_tensor(f"{name}_{uid[0]}", list(shape), dt))

        ps = [es.enter_context(nc.psum_tensor(f"ps{i}", [128, 512], F32)) for i in range(8)]
        psk = [("ps", i) for i in range(8)]
        rr = {"a": 0, "b": 0, "c": 0, "d": 0}

        def bank(role):
            base = {"a": 0, "b": 2, "c": 4, "d": 6}[role]
            i = base + rr[role] % 2
            rr[role] += 1
            return i

        h = sb(es, "h", [128, 8, T])
        u = sb(es, "u", [128, 8, T], BF16)
        cos = sb(es, "cos", [128, SEQ])
        sin = sb(es, "sin", [128, SEQ])
        cf = sb(es, "cf", [128, 4, 128])
        cb16 = sb(es, "cb16", [128, 2, 128], BF16)
        mod = sb(es, "mod", [128, DEPTH, 48, 3])
        lnp = sb(es, "lnp", [128, DEPTH, 4, 8])
        convp = sb(es, "convp", [128, DEPTH, 4, 5])
        rgv = sb(es, "rgv", [128, DEPTH, 2, 4, 3])
        rgc = sb(es, "rgc", [128, DEPTH, 2, 4])
        subg = sb(es, "subg", [128, DEPTH])
        lamv = sb(es, "lamv", [128, DEPTH, 4])
        rout = sb(es, "rout", [128, 2, 8, NE])
        ident = cf[:, 0, :]
        ones_d = cf[:, 1, :]
        ones_h = cf[:, 2, :]
        ones_1 = cf[:, 3, :]
        onesb = cb16[:, 0, :]
        Rb = cb16[:, 1, :]

        S.dma("sp", "const", [(cos[:], cosd[:, :]), (sin[:], sind[:, :]), (cf[:], constf[:, :, :]),
                              (cb16[:], constb[:, :, :]), (lnp[:], lnT[:, :, :, :]), (convp[:], convT[:, :, :, :]),
                              (rgv[:], rgvT[:, :, :, :, :]), (subg[:], sublnT[:, :]),
                              (rout[:], routerT[:, :, :, :])],
              writes=["const"])

        def M(l, q, n):
            return mod[:, l, q, n:n + 1]

        def load_h(s):
            xv = xT[s].rearrange("(c p) t -> p c t", p=128)
            cv = ctxT[s].rearrange("(c p) t -> p c t", p=128)
            S.dma("pool", "hld", [(h[:, 0:4, CTX:], xv[:, 0:4, :]), (h[:, 4:8, CTX:], xv[:, 4:8, :]), (h[:, :, 0:CTX], cv[:, :, :])],
                  writes=[("h", c, t) for c in range(8) for t in range(5)])
        load_h(0)

        with ExitStack() as ph:
            scT = sb(ph, "scT", [128, 8, 3])
            bmod = sb(ph, "bmod", [128, DEPTH, 48])
            wm = [sb(ph, f"wm{i}", [128, 8, 512]) for i in range(2)]
            lamt = sb(ph, "lamt", [128, DEPTH, 4, 64])
            S.dma("sp", "cld", [(scT[:], cT[:, :, :]), (bmod[:], b_modT[:, :, :]), (lamt[:], lamqk[:, :, :, :])], writes=["scT", "bmod", "lamt"])
            S.op("act", lambda e: e.activation(out=scT[:], in_=scT[:], func=AF.Silu), reads=["scT"], writes=["scT"])
            it = 0
            for l in range(NL):
                wv_ = w_mod[l].rearrange("(c p) f -> p c f", p=128)
                for j in range(12):
                    sl = it % 2
                    it += 1
                    S.dma("sp", f"wm{sl}", [(wm[sl][:, 0:4, :], wv_[:, 0:4, j * 512:(j + 1) * 512]),
                                            (wm[sl][:, 4:8, :], wv_[:, 4:8, j * 512:(j + 1) * 512])],
                          writes=[("wm", sl)])
                    for fc in range(4):
                        b_ = bank("d")
                        for k in range(8):
                            S.op("pe", lambda e: e.matmul(ps[b_][:, 0:3], wm[sl][:, k, fc * 128:(fc + 1) * 128], scT[:, k, :],
                                                          start=(k == 0), stop=(k == 7)),
                                 reads=[("wm", sl), "scT"], writes=[psk[b_]])
                        q = j * 4 + fc
                        S.op("dve", lambda e: e.tensor_scalar(mod[:, l, q, :], ps[b_][:, 0:3], bmod[:, l, q:q + 1], None, ALU.add),
                             reads=[psk[b_], "bmod"], writes=["mod"])
                for g0 in (8, 32):
                    S.op("dve", lambda e: e.tensor_scalar(mod[:, l, g0:g0 + 8, :], mod[:, l, g0:g0 + 8, :], 1.0, None, ALU.add),
                         reads=["mod"], writes=["mod"])
                for g0 in (16, 40):
                    S.op("dve", lambda e: e.tensor_scalar(mod[:, l, g0:g0 + 8, :], mod[:, l, g0:g0 + 8, :], 1.0, 1.0 / ALPHA,
                                                          ALU.add, ALU.mult),
                         reads=["mod"], writes=["mod"])
            S.op("act", lambda e: e.activation(out=rgc[:], in_=rgv[:, :, :, :, 2], func=AF.Exp, scale=-1.0),
                 reads=["const"], writes=["rgc"])
            S.op("dve", lambda e: e.tensor_scalar(rgc[:], rgc[:], 1.0, None, ALU.add), reads=["rgc"], writes=["rgc"])
            S.op("act", lambda e: e.activation(out=rgc[:], in_=rgc[:], func=AF.Ln), reads=["rgc"], writes=["rgc"])
            S.op("dve", lambda e: e.tensor_scalar(rgc[:], rgc[:], -8.0, None, ALU.mult), reads=["rgc"], writes=["rgc"])
            for l in range(NL):
                lam_init = 0.8 - 0.6 * math.exp(-0.3 * l)
                S.op("dve", lambda e: e.tensor_tensor(lamt[:, l, 0, :], lamt[:, l, 0, :], lamt[:, l, 1, :], ALU.mult),
                     reads=["lamt"], writes=["lamt"])
                S.op("dve", lambda e: e.tensor_tensor(lamt[:, l, 2, :], lamt[:, l, 2, :], lamt[:, l, 3, :], ALU.mult),
                     reads=["lamt"], writes=["lamt"])
                S.op("dve", lambda e: e.reduce_sum(lamv[:, l, 0:1], lamt[:, l, 0, :], AX.X), reads=["lamt"], writes=["lamv"])
                S.op("dve", lambda e: e.reduce_sum(lamv[:, l, 1:2], lamt[:, l, 2, :], AX.X), reads=["lamv", "lamt"], writes=["lamv"])
                S.op("act", lambda e: e.activation(out=lamv[:, l, 0:2], in_=lamv[:, l, 0:2], func=AF.Exp),
                     reads=["lamv"], writes=["lamv"])
                S.op("dve", lambda e: e.scalar_tensor_tensor(lamv[:, l, 2:3], lamv[:, l, 1:2], -lam_init, lamv[:, l, 0:1],
                                                             ALU.add, ALU.subtract),
                     reads=["lamv"], writes=["lamv"])
                S.op("dve", lambda e: e.tensor_scalar(subg[:, l:l + 1], subg[:, l:l + 1], 1.0 - lam_init, None, ALU.mult),
                     reads=["const", "lamv"], writes=["subg"])
            S.barrier()

        def tile_n(t, s):
            return 2 if t == 0 else s

        def hk(c, t):
            return ("h", c, t)

        def uk(t):
            return ("u", t)

        def out_proj_partial(l, s, t, wo, wokey, mixt, mixkey, role="c", evs=None):
            t0, tn = TILES[t]
            n = tile_n(t, s)
            for m in range(8):
                if evs is not None and m % 3 == 1:
                    b_ = bank("d")
                    S.op("pe", lambda e: e.matmul(ps[b_][:, :tn], wo[:, m * 128:(m + 1) * 128], mixt[:, :tn], start=True, stop=True),
                         reads=[wokey, mixkey], writes=[psk[b_]])
                    ev_, evk = evs[m % 2], ("oevt", m % 2)
                    S.op("act", lambda e: e.activation(out=ev_[:, :tn], in_=ps[b_][:, :tn], func=AF.Identity, scale=M(l, 16 + m, n)),
                         reads=[psk[b_], "mod"], writes=[evk])
                    S.op("pool", lambda e: e.tensor_tensor(h[:, m, t0:t0 + tn], h[:, m, t0:t0 + tn], ev_[:, :tn], ALU.add),
                         reads=[evk, hk(m, t)], writes=[hk(m, t)])
                    continue
                b_ = bank(role)
                S.op("pe", lambda e: e.matmul(ps[b_][:, :tn], wo[:, m * 128:(m + 1) * 128], mixt[:, :tn], start=True, stop=True),
                     reads=[wokey, mixkey], writes=[psk[b_]])
                S.op("dve", lambda e: e.scalar_tensor_tensor(h[:, m, t0:t0 + tn], ps[b_][:, :tn], M(l, 16 + m, n),
                                                             h[:, m, t0:t0 + tn], ALU.mult, ALU.add),
                     reads=[psk[b_], hk(m, t)], writes=[hk(m, t)])

        def layer_norm(l, s, which, tiles, nxt, ph, router_j=None, lg=None):
            gq, bq = (0, 1) if which == 1 else (2, 3)
            tmp = [sb(ph, f"ln_tmp{i}", [128, 512]) for i in range(2)]
            msb = sb(ph, "ln_m", [128, 512])
            rstd = sb(ph, "ln_r", [128, 512])
            sq = [sb(ph, f"ln_sq{i}", [128, 512]) for i in range(2)]
            u32 = [sb(ph, f"ln_u32{i}", [128, 512]) for i in range(2)] if router_j is not None else None
            lgT = sb(ph, "ln_lgT", [8, 512]) if router_j is not None else None
            for t in tiles:
                t0, tn = TILES[t]
                n = tile_n(t, s)
                bm, bv = bank("d"), bank("d")
                for c in range(8):
                    S.op("pe", lambda e: e.matmul(ps[bm][:, :tn], ones_d, h[:, c, t0:t0 + tn], start=(c == 0), stop=(c == 7)),
                         reads=[hk(c, t), "const"], writes=[psk[bm]])
                for c in range(8):
                    sq_ = sq[c % 2]
                    S.op("act", lambda e: e.activation(out=sq_[:, :tn], in_=h[:, c, t0:t0 + tn], func=AF.Square),
                         reads=[hk(c, t)], writes=[("lnsq", c % 2)])
                    S.op("pe", lambda e: e.matmul(ps[bv][:, :tn], ones_d, sq_[:, :tn], start=(c == 0), stop=(c == 7)),
                         reads=[("lnsq", c % 2), "const"], writes=[psk[bv]])
                S.op("act", lambda e: e.activation(out=msb[:, :tn], in_=ps[bm][:, :tn], func=AF.Identity), reads=[psk[bm]], writes=["lnm"])
                S.op("dve", lambda e: e.tensor_tensor(rstd[:, :tn], msb[:, :tn], msb[:, :tn], ALU.mult), reads=["lnm"], writes=["lnr"])
                S.op("dve", lambda e: e.scalar_tensor_tensor(rstd[:, :tn], ps[bv][:, :tn], LN_EPS, rstd[:, :tn], ALU.add, ALU.subtract),
                     reads=[psk[bv], "lnr"], writes=["lnr"])
                S.op("act", lambda e: e.activation(out=rstd[:, :tn], in_=rstd[:, :tn], func=AF.Ln), reads=["lnr"], writes=["lnr"])
                S.op("act", lambda e: e.activation(out=rstd[:, :tn], in_=rstd[:, :tn], func=AF.Exp, scale=-0.5), reads=["lnr"], writes=["lnr"])
                if router_j is not None:
                    bl = bank("c")
                for c in range(8):
                    tm = tmp[c % 2]
                    hv = h[:, c, t0:t0 + tn]
                    S.op("dve", lambda e: e.tensor_tensor(tm[:, :tn], hv, msb[:, :tn], ALU.subtract),
                         reads=[hk(c, t), "lnm"], writes=[("lntmp", c % 2)])
                    S.op("dve", lambda e: e.tensor_tensor(tm[:, :tn], tm[:, :tn], rstd[:, :tn], ALU.mult),
                         reads=[("lntmp", c % 2), "lnr"], writes=[("lntmp", c % 2)])
                    S.op("dve", lambda e: e.tensor_scalar(hv, tm[:, :tn], lnp[:, l, gq, c:c + 1], lnp[:, l, bq, c:c + 1], ALU.mult, ALU.add),
                         reads=[("lntmp", c % 2), "const"], writes=[hk(c, t)])
                    if nxt is not None:
                        l2, oq, sq0 = nxt
                        S.op("act", lambda e: e.activation(out=u[:, c, t0:t0 + tn], in_=hv, func=AF.Identity,
                                                           bias=M(l2, sq0 + c, n), scale=M(l2, oq + c, n)),
                             reads=[hk(c, t), "mod"], writes=[uk(t)])
                        if router_j is not None:
                            uu = u32[c % 2]
                            S.op("dve", lambda e: e.tensor_scalar(uu[:, :tn], hv, M(l2, oq + c, n), M(l2, sq0 + c, n), ALU.mult, ALU.add),
                                 reads=[hk(c, t), "mod"], writes=[("u32", c % 2)])
                            S.op("pe", lambda e: e.matmul(ps[bl][0:8, :tn], rout[:, router_j, c, :], uu[:, :tn], start=(c == 0), stop=(c == 7)),
                                 reads=[("u32", c % 2), "const"], writes=[psk[bl]])
                if router_j is not None:
                    S.op("act", lambda e: e.activation(out=lgT[:, :tn], in_=ps[bl][0:8, :tn], func=AF.Identity), reads=[psk[bl]], writes=["lgT"])
                    for q in range(tn // 128):
                        b2 = bank("d")
                        S.op("pe", lambda e: e.matmul(ps[b2][:, 0:8], lgT[:, q * 128:(q + 1) * 128], ident[0:8, 0:8], start=True, stop=True),
                             reads=["lgT", "const"], writes=[psk[b2]])
                        tt = t0 // 128 + q
                        S.op("dve", lambda e: e.tensor_copy(lg[:, tt, :], ps[b2][:, 0:8]), reads=[psk[b2]], writes=["lg"])

        for s in range(NSEQ):
            if s > 0:
                load_h(s)
            for t in range(5):
                t0, tn = TILES[t]
                n = tile_n(t, s)
                for c in range(8):
                    S.op("dve", lambda e: e.tensor_scalar(u[:, c, t0:t0 + tn], h[:, c, t0:t0 + tn], M(0, 8 + c, n), M(0, c, n), ALU.mult, ALU.add),
                         reads=[hk(c, t), "mod"], writes=[uk(t)])

            for l in range(NL):
                last = (l == DEPTH - 1)
                qtiles = [1, 2, 3, 4] if last else [0, 1, 2, 3, 4]
                win = w_in[l].rearrange("(c p) f -> p c f", p=128)
                wout = w_out[l]

                with ExitStack() as ph:
                    xrp = sb(ph, "xrp", [128, T + 8])
                    xc = sb(ph, "xc", [128, T])
                    A = sb(ph, "A", [128, T])
                    Bm = sb(ph, "Bm", [128, T])
                    H = sb(ph, "H", [128, T])
                    wxr = [sb(ph, f"wxr{i}", [128, 8, 128], BF16) for i in range(2)]
                    wy = [sb(ph, f"wy{i}", [128, 8, 128], BF16) for i in range(2)]
                    wo = [sb(ph, f"wo{i}", [128, D], BF16) for i in range(2)]
                    wg = [sb(ph, f"wg{i}", [128, 4, 128]) for i in range(2)]
                    tmpf = [sb(ph, f"rtmp{i}", [128, 512]) for i in range(2)]
                    oev = [sb(ph, f"oev{i}", [128, 512]) for i in range(2)]
                    mixt = [sb(ph, f"rmix{i}", [128, 512], BF16) for i in range(2)]
                    mi = 0
                    CO, LO = 2, 261

                    def load_rg(j):
                        sl = j % 2
                        S.dma("pool", f"rgwp{sl}", [(wxr[sl][:], win[:, :, 1024 + j * 128:1024 + (j + 1) * 128]),
                                                    (wy[sl][:], win[:, :, 2048 + j * 128:2048 + (j + 1) * 128]),
                                                    (wo[sl][:], wout[(4 + j) * 128:(5 + j) * 128, :])],
                              writes=[("rgwp", sl)])
                        S.dma("sp", f"rgws{sl}", [(wg[sl][:, d * 2 + g, :], gate_bd[l, d, j, g]) for d in range(2) for g in range(2)],
                              writes=[("rgws", sl)])
                    if run("rg"):
                        load_rg(0)
                    for j in range(4 if run("rg") else 0):
                        sl = j % 2
                        if j + 1 < 4:
                            load_rg(j + 1)
                        S.op("pool", lambda e: e.memset(xrp[:, 0:2], 0.0), writes=["xrp"])
                        S.op("pool", lambda e: e.memset(xrp[:, 258:261], 0.0), reads=["xrp"], writes=["xrp"])
                        S.op("pool", lambda e: e.memset(xrp[:, 2309:2312], 0.0), reads=["xrp"], writes=["xrp"])
                        for t in range(5):
                            t0, tn = TILES[t]
                            b_ = bank("a")
                            for k in range(8):
                                S.op("pe", lambda e: e.matmul(ps[b_][:, :tn], wxr[sl][:, k, :], u[:, k, t0:t0 + tn], start=(k == 0), stop=(k == 7)),
                                     reads=[("rgwp", sl), uk(t)], writes=[psk[b_]])
                            o0 = CO if t == 0 else LO + (t0 - CTX)
                            S.op("act", lambda e: e.activation(out=xrp[:, o0:o0 + tn], in_=ps[b_][:, :tn], func=AF.Identity), reads=[psk[b_], "xrp"], writes=["xrp"])
                        for (dst0, n_, src0) in ((0, CTX, CO - 2), (CTX, SEQ, LO - 2)):
                            S.op("dve", lambda e: e.tensor_scalar(xc[:, dst0:dst0 + n_], xrp[:, src0:src0 + n_], convp[:, l, j, 0:1],
                                                                  convp[:, l, j, 4:5], ALU.mult, ALU.add),
                                 reads=["xrp", "const"], writes=["xc"])
                            for tap in range(1, 4):
                                S.op("dve", lambda e: e.scalar_tensor_tensor(xc[:, dst0:dst0 + n_], xrp[:, src0 + tap:src0 + tap + n_],
                                                                             convp[:, l, j, tap:tap + 1], xc[:, dst0:dst0 + n_], ALU.mult, ALU.add),
                                     reads=["xrp", "xc"], writes=["xc"])
                        for d in range(2):
                            for t in range(5):
                                t0, tn = TILES[t]
                                for g, dst, key in ((0, A, "A"), (1, Bm, "B")):
                                    b_ = bank("b")
                                    S.op("pe", lambda e: e.matmul(ps[b_][:, :tn], wg[sl][:, d * 2 + g, :], xc[:, t0:t0 + tn], start=True, stop=True),
                                         reads=[("rgws", sl), "xc"], writes=[psk[b_]])
                                    S.op("act", lambda e: e.activation(out=dst[:, t0:t0 + tn], in_=ps[b_][:, :tn], func=AF.Sigmoid,
                                                                       bias=rgv[:, l, d, j, g:g + 1]),
                                         reads=[psk[b_], "const"], writes=[key])
                            S.op("act", lambda e: e.activation(out=A[:], in_=A[:], func=AF.Exp, scale=rgc[:, l, d, j:j + 1]),
                                 reads=["A", "rgc"], writes=["A"])
                            S.op("dve", lambda e: e.tensor_tensor(Bm[:], Bm[:], xc[:], ALU.mult), reads=["B", "xc"], writes=["B"])
                            scr = xrp[:, 0:T]
                            S.op("act", lambda e: e.activation(out=scr, in_=A[:], func=AF.Square), reads=["A", "xrp"], writes=["xrp"])
                            S.op("dve", lambda e: e.tensor_scalar(scr, scr, -1.0, 1.0, ALU.mult, ALU.add), reads=["xrp"], writes=["xrp"])
                            S.op("dve", lambda e: e.tensor_scalar(scr, scr, 0.0, None, ALU.max), reads=["xrp"], writes=["xrp"])
                            S.op("act", lambda e: e.activation(out=scr, in_=scr, func=AF.Sqrt), reads=["xrp"], writes=["xrp"])
                            S.op("dve", lambda e: e.tensor_tensor(Bm[:], Bm[:], scr, ALU.mult), reads=["B", "xrp"], writes=["B"])
                            if d == 0:
                                S.op("dve", lambda e: e.tensor_tensor_scan(H[:, 0:CTX], A[:, 0:CTX], Bm[:, 0:CTX], 0.0, ALU.mult, ALU.add),
                                     reads=["A", "B"], writes=["H"])
                                S.op("dve", lambda e: e.tensor_tensor_scan(H[:, CTX:], A[:, CTX:], Bm[:, CTX:], H[:, CTX - 1:CTX], ALU.mult, ALU.add),
                                     reads=["A", "B", "H"], writes=["H"])
                            else:
                                C = xrp
                                S.op("dve", lambda e: e.tensor_tensor_scan(C[:, 0:CTX][:, ::-1], A[:, 0:CTX][:, ::-1], Bm[:, 0:CTX][:, ::-1],
                                                                           0.0, ALU.mult, ALU.add),
                                     reads=["A", "B", "xrp"], writes=["xrp"])
                                S.op("dve", lambda e: e.tensor_tensor_scan(C[:, CTX:T][:, ::-1], A[:, CTX:][:, ::-1], Bm[:, CTX:][:, ::-1],
                                                                           C[:, 0:1], ALU.mult, ALU.add),
                                     reads=["A", "B", "xrp"], writes=["xrp"])
                                S.op("dve", lambda e: e.tensor_tensor(H[:], H[:], C[:, 0:T], ALU.add), reads=["H", "xrp"], writes=["H"])
                        for t in qtiles:
                            t0, tn = TILES[t]
                            b_ = bank("a")
                            for k in range(8):
                                S.op("pe", lambda e: e.matmul(ps[b_][:, :tn], wy[sl][:, k, :], u[:, k, t0:t0 + tn], start=(k == 0), stop=(k == 7)),
                                     reads=[("rgwp", sl), uk(t)], writes=[psk[b_]])
                            tf, mx = tmpf[mi % 2], mixt[mi % 2]
                            kf, km = ("rtmp", mi % 2), ("rmix", mi % 2)
                            mi += 1
                            S.op("act", lambda e: e.activation(out=tf[:, :tn], in_=ps[b_][:, :tn], func=AF.Gelu_apprx_tanh),
                                 reads=[psk[b_]], writes=[kf])
                            S.op("dve", lambda e: e.tensor_tensor(mx[:, :tn], tf[:, :tn], H[:, t0:t0 + tn], ALU.mult), reads=[kf, "H"], writes=[km])
                            out_proj_partial(l, s, t, wo[sl], ("rgwp", sl), mx, km, evs=oev)
                    S.barrier()

                with ExitStack() as ph:
                    v = sb(ph, "v", [128, 18, 512], BF16)
                    wk = [sb(ph, f"wk{i}", [128, 8, 128], BF16) for i in range(2)]
                    wq = [sb(ph, f"wq{i}", [128, 8, 128], BF16) for i in range(2)]
                    wo = [sb(ph, f"awo{i}", [128, D], BF16) for i in range(2)]

                    def load_head(hd):
                        sl = hd % 2
                        S.dma("pool", f"attw{sl}", [(wk[sl][:], win[:, :, hd * 128:(hd + 1) * 128]),
                                                    (wq[sl][:], win[:, :, 1536 + hd * 128:1536 + (hd + 1) * 128]),
                                                    (wo[sl][:], wout[hd * 128:(hd + 1) * 128, :])],
                              writes=[("attw", sl)])
                    with ExitStack() as phv:
                        wv = sb(phv, "wv", [128, 8, 512], BF16)
                        S.dma("pool", "wv", [(wv[:], win[:, :, 512:1024])], writes=["wv"])
                        load_head(0)
                        for tt in range(18):
                            b_ = bank("a")
                            tile_of = 0 if tt < 2 else 1 + (tt - 2) // 4
                            for k in range(8):
                                S.op("pe", lambda e: e.matmul(ps[b_][:, :], u[:, k, tt * 128:(tt + 1) * 128], wv[:, k, :], start=(k == 0), stop=(k == 7)),
                                     reads=["wv", uk(tile_of)], writes=[psk[b_]])
                            S.op("act", lambda e: e.activation(out=v[:, tt, :], in_=ps[b_][:, :], func=AF.Identity), reads=[psk[b_]], writes=["v"])
                        S.barrier()
                    kh = [sb(ph, f"kh{i}", [128, T], BF16) for i in range(2)]
                    qz = [[sb(ph, f"qz{i}_{j}", [128, T], BF16) for j in range(2)] for i in range(2)]
                    E = [sb(ph, f"E{i}", [128, 512], BF16) for i in range(3)]
                    qb = [sb(ph, f"qb{i}", [128, 512], BF16) for i in range(2)]
                    rm = [sb(ph, f"rm{i}", [128, 512]) for i in range(2)]
                    at = sb(ph, "at", [128, 512])
                    sqt = sb(ph, "sqt", [128, 512])
                    r1t = sb(ph, "r1t", [128, 512])
                    r2t = sb(ph, "r2t", [128, 512])
                    mixt = [sb(ph, f"amix{i}", [128, 512], BF16) for i in range(2)]
                    for i in range(2):
                        S.op("pool", lambda e: e.memset(qz[i][0][64:128, :], 0.0), writes=[("qh", i)])
                        S.op("pool", lambda e: e.memset(qz[i][1][0:64, :], 0.0), writes=[("qh", i)])
                    qi = 0
                    mi = 0

                    def proj_task(hd, is_q, t):
                        sl, par = hd % 2, hd % 2
                        wt = wq[sl] if is_q else wk[sl]
                        dkey = ("qh", par) if is_q else ("kh", par)
                        t0, tn = TILES[t]
                        b_ = 3
                        st = {}

                        def sA():
                            for k in range(8):
                                S.op("pe", lambda e: e.matmul(ps[b_][:, :tn], wt[:, k, :], u[:, k, t0:t0 + tn], start=(k == 0), stop=(k == 7)),
                                     reads=[("attw", sl), uk(t)], writes=[psk[b_]])

                        def sB():
                            nonlocal qi
                            if t == 0:
                                if is_q:
                                    S.op("act", lambda e: e.activation(out=qz[par][0][0:64, t0:t0 + tn], in_=ps[b_][0:64, :tn], func=AF.Identity),
                                         reads=[psk[b_]], writes=[dkey])
                                    S.op("act", lambda e: e.activation(out=qz[par][1][64:128, t0:t0 + tn], in_=ps[b_][64:128, :tn], func=AF.Identity),
                                         reads=[psk[b_]], writes=[dkey])
                                else:
                                    S.op("act", lambda e: e.activation(out=kh[par][:, t0:t0 + tn], in_=ps[b_][:, :tn], func=AF.Identity),
                                         reads=[psk[b_]], writes=[dkey])
                                return
                            st["i2"] = qi % 2
                            qi += 1
                            i2 = st["i2"]
                            S.op("act", lambda e: e.activation(out=qb[i2][:, :tn], in_=ps[b_][:, :tn], func=AF.Identity), reads=[psk[b_]], writes=[("qb", i2)])

                        def sC():
                            if t == 0:
                                return
                            i2 = st["i2"]
                            l0 = t0 - CTX
                            b2 = bank("d")
                            st["b2"] = b2
                            S.op("pe", lambda e: e.matmul(ps[b2][:, :tn], Rb, qb[i2][:, :tn], start=True, stop=True),
                                 reads=[("qb", i2), "const"], writes=[psk[b2]])
                            S.op("dve", lambda e: e.tensor_tensor(r1t[:, :tn], cos[:, l0:l0 + tn], ps[b_][:, :tn], ALU.mult),
                                 reads=[psk[b_], "const"], writes=["r1t"])
                            S.op("dve", lambda e: e.tensor_tensor(r2t[:, :tn], sin[:, l0:l0 + tn], ps[b2][:, :tn], ALU.mult),
                                 reads=[psk[b2], "const"], writes=["r2t"])

                        def sD():
                            if t == 0:
                                return
                            if is_q:
                                S.op("dve", lambda e: e.tensor_tensor(qz[par][0][0:64, t0:t0 + tn], r1t[0:64, :tn], r2t[0:64, :tn], ALU.add),
                                     reads=["r1t", "r2t"], writes=[dkey])
                                S.op("dve", lambda e: e.tensor_tensor(qz[par][1][64:128, t0:t0 + tn], r1t[64:128, :tn], r2t[64:128, :tn], ALU.add),
                                     reads=["r1t", "r2t"], writes=[dkey])
                            else:
                                S.op("dve", lambda e: e.tensor_tensor(kh[par][:, t0:t0 + tn], r1t[:, :tn], r2t[:, :tn], ALU.add),
                                     reads=["r1t", "r2t"], writes=[dkey])
                        return [(0, sA), (4, sB), (6, sC), (8, sD)]

                    def head_tasks(hd):
                        return [proj_task(hd, False, t) for t in range(5)] + [proj_task(hd, True, t) for t in qtiles]

                    LA = 2
                    steps = []
                    head_start = {}
                    for hd in range(4):
                        head_start[hd] = len(steps)
                        for t in qtiles:
                            kts = [0, 1] if t == 0 else list(range(18))
                            for mp in range(2):
                                for ki, kt in enumerate(kts):
                                    steps.append((hd, t, mp, ki, kt, len(kts)))
                    bo, bd = 4, 5
                    deferred = {}

                    def defer(step, fn):
                        deferred.setdefault(step, []).append(fn)
                    for stages in head_tasks(0):
                        for _, fn in stages:
                            fn()
                    sched = {}
                    for hd in range(1, 4):
                        st0 = head_start[hd - 1]
                        sched.setdefault(st0 + 26, []).append((lambda hd_: (lambda: load_head(hd_)))(hd))
                        for n_, stages in enumerate(head_tasks(hd)):
                            for dl, fn in stages:
                                sched.setdefault(st0 + 30 + 10 * n_ + dl, []).append(fn)

                    def s_exp(i):
                        hd, t, mp, ki, kt, nk = steps[i]
                        par = hd % 2
                        t0, tn = TILES[t]
                        b_ = i % 3
                        S.op("pe", lambda e: e.matmul(ps[b_][:, :tn], kh[par][:, kt * 128:(kt + 1) * 128], qz[par][mp][:, t0:t0 + tn],
                                                      start=True, stop=True),
                             reads=[("kh", par), ("qh", par)], writes=[psk[b_]])
                        Et, ek = E[i % 3], ("E", i % 3)
                        S.op("act", lambda e: e.activation(out=Et[:, :tn], in_=ps[b_][:, :tn], func=AF.Exp, scale=ATTN_SCALE),
                             reads=[psk[b_]], writes=[ek])

                    def pv_d(i):
                        nonlocal mi
                        hd, t, mp, ki, kt, nk = steps[i]
                        sl = hd % 2
                        t0, tn = TILES[t]
                        Et, ek = E[i % 3], ("E", i % 3)
                        S.op("pe", lambda e: e.matmul(ps[bo][:, :tn], v[:, kt, hd * 128:(hd + 1) * 128], Et[:, :tn],
                                                      start=(ki == 0), stop=(ki == nk - 1)),
                             reads=[ek, "v"], writes=[psk[bo]])
                        S.op("pe", lambda e: e.matmul(ps[bd][:, :tn], onesb, Et[:, :tn],
                                                      start=(ki == 0), stop=(ki == nk - 1)),
                             reads=[ek, "const"], writes=[psk[bd]])
                        if ki != nk - 1:
                            return
                        S.op("act", lambda e: e.activation(out=rm[mp][:, :tn], in_=ps[bd][:, :tn], func=AF.Ln), reads=[psk[bd]], writes=[("rm", mp)])
                        S.op("act", lambda e: e.activation(out=rm[mp][:, :tn], in_=rm[mp][:, :tn], func=AF.Exp, scale=-1.0),
                             reads=[("rm", mp)], writes=[("rm", mp)])
                        S.op("dve", lambda e: e.tensor_tensor(rm[mp][:, :tn], ps[bo][:, :tn], rm[mp][:, :tn], ALU.mult),
                             reads=[psk[bo], ("rm", mp)], writes=[("rm", mp)])
                        if mp == 0:
                            return
                        if t == 0:
                            for k_ in sorted(deferred):
                                for fn in deferred.pop(k_):
                                    fn()
                        S.op("dve", lambda e: e.scalar_tensor_tensor(at[:, :tn], rm[1][:, :tn], lamv[:, l, 2:3], rm[0][:, :tn], ALU.mult, ALU.add),
                             reads=[("rm", 0), ("rm", 1), "lamv"], writes=["at"])
                        S.op("act", lambda e: e.activation(out=sqt[:, :tn], in_=at[:, :tn], func=AF.Square), reads=["at"], writes=["sqt"])
                        mx, km = mixt[mi % 2], ("amix", mi % 2)
                        mi += 1
                        n = tile_n(t, s)

                        def stage2():
                            b_ = bank("d")
                            S.op("pe", lambda e: e.matmul(ps[b_][:, :tn], ones_h, sqt[:, :tn], start=True, stop=True),
                                 reads=["sqt", "const"], writes=[psk[b_]])
                            S.op("dve", lambda e: e.tensor_scalar(sqt[:, :tn], ps[b_][:, :tn], RMS_EPS, None, ALU.add), reads=[psk[b_], "sqt"], writes=["sqt"])
                            S.op("act", lambda e: e.activation(out=sqt[:, :tn], in_=sqt[:, :tn], func=AF.Ln), reads=["sqt"], writes=["sqt"])
                            S.op("act", lambda e: e.activation(out=sqt[:, :tn], in_=sqt[:, :tn], func=AF.Exp, scale=-0.5), reads=["sqt"], writes=["sqt"])
                            S.op("dve", lambda e: e.scalar_tensor_tensor(mx[:, :tn], at[:, :tn], subg[:, l:l + 1], sqt[:, :tn], ALU.mult, ALU.mult),
                                 reads=["at", "sqt", "subg"], writes=[km])

                        def stage3(m):
                            def f():
                                b_ = bank("d")
                                S.op("pe", lambda e: e.matmul(ps[b_][:, :tn], wo[sl][:, m * 128:(m + 1) * 128], mx[:, :tn], start=True, stop=True),
                                     reads=[("attw", sl), km], writes=[psk[b_]])
                                S.op("dve", lambda e: e.scalar_tensor_tensor(h[:, m, t0:t0 + tn], ps[b_][:, :tn], M(l, 16 + m, n),
                                                                             h[:, m, t0:t0 + tn], ALU.mult, ALU.add),
                                     reads=[psk[b_], hk(m, t)], writes=[hk(m, t)])
                            return f
                        cur = i + LA
                        if t == 0:
                            stage2()
                            for m in range(8):
                                stage3(m)()
                            return
                        defer(cur + 6, stage2)
                        for m in range(8):
                            defer(cur + 13 + m, stage3(m))
                    nst = len(steps)
                    i = 0
                    while i < nst + LA or deferred or sched:
                        if i < nst:
                            s_exp(i)
                        if 0 <= i - LA < nst:
                            pv_d(i - LA)
                        for fn in deferred.pop(i, []):
                            fn()
                        for fn in sched.pop(i, []):
                            fn()
                        i += 1
                    S.barrier()

                moe = (l % 2 == 1)
                jj = l // 2
                with ExitStack() as ph2:
                    lg = sb(ph2, "lg", [128, 18, NE]) if moe else None
                    comb = sb(ph2, "comb", [128, 18, NE]) if moe else None
                    with ExitStack() as ph:
                        if run("ln1"):
                            layer_norm(l, s, 1, qtiles, (l, 32, 24), ph, router_j=(jj if moe else None), lg=lg)
                        S.barrier()

                    with ExitStack() as ph:
                        w1b = [sb(ph, f"w1b{i}", [128, 8, 256], BF16) for i in range(2)]
                        w3b = [sb(ph, f"w3b{i}", [128, 8, 256], BF16) for i in range(2)]
                        w2b = [sb(ph, f"w2b{i}", [128, 2, D], BF16) for i in range(2)]
                        act_ = [sb(ph, f"act{i}", [128, 2, T], BF16) for i in range(2)]
                        st_ = [sb(ph, f"st{i}", [128, 512]) for i in range(2)]
                        evt = [sb(ph, f"evt{i}", [128, 512]) for i in range(2)]
                        dj = 0
                        ej = 0
                        dvr = 0
                        pt_ = [sb(ph, f"pt{i}", [128, 512]) for i in range(2)] if moe else None
                        cbt = [sb(ph, f"cbt{i}", [128, T]) for i in range(2)] if moe else None
                        dg = [sb(ph, f"dg{i}", [128, 128]) for i in range(2)] if moe else None
                        sm = sb(ph, "sm", [128, 8]) if moe else None
                        if moe and run("ffn"):
                            for tt in (range(18) if not last else range(2, 18)):
                                L = lg[:, tt, :]
                                Cb = comb[:, tt, :]
                                S.op("dve", lambda e: e.reduce_max(sm[:, 0:1], L, AX.X), reads=["lg", "sm"], writes=["sm"])
                                S.op("dve", lambda e: e.tensor_scalar(Cb, L, sm[:, 0:1], None, ALU.is_equal), reads=["lg", "sm"], writes=["comb"])
                                S.op("dve", lambda e: e.scalar_tensor_tensor(L, Cb, -1e30, L, ALU.mult, ALU.add), reads=["comb", "lg"], writes=["lg"])
                                S.op("dve", lambda e: e.reduce_max(sm[:, 1:2], L, AX.X), reads=["lg", "sm"], writes=["sm"])
                                S.op("dve", lambda e: e.tensor_scalar(L, L, sm[:, 1:2], None, ALU.is_equal), reads=["lg", "sm"], writes=["lg"])
                                S.op("dve", lambda e: e.tensor_tensor(sm[:, 2:3], sm[:, 1:2], sm[:, 0:1], ALU.subtract), reads=["sm"], writes=["sm"])
                                S.op("act", lambda e: e.activation(out=sm[:, 3:4], in_=sm[:, 2:3], func=AF.Exp), reads=["sm"], writes=["sm"])
                                S.op("dve", lambda e: e.tensor_scalar(sm[:, 4:5], sm[:, 3:4], 1.0, None, ALU.add), reads=["sm"], writes=["sm"])
                                S.op("dve", lambda e: e.reciprocal(sm[:, 5:6], sm[:, 4:5]), reads=["sm"], writes=["sm"])
                                S.op("dve", lambda e: e.tensor_tensor(sm[:, 6:7], sm[:, 3:4], sm[:, 5:6], ALU.mult), reads=["sm"], writes=["sm"])
                                S.op("dve", lambda e: e.tensor_scalar(Cb, Cb, sm[:, 5:6], None, ALU.mult), reads=["comb", "sm"], writes=["comb"])
                                S.op("dve", lambda e: e.scalar_tensor_tensor(Cb, L, sm[:, 6:7], Cb, ALU.mult, ALU.add), reads=["comb", "lg", "sm"], writes=["comb"])
                        experts = range(NE) if moe else range(1)
                        gi = 0

                        def wsrc(e_):
                            if moe:
                                return moe_w1[jj, e_], moe_w3[jj, e_], moe_w2[jj, e_]
                            return ffn_w1[jj], ffn_w3[jj], ffn_w2[jj]

                        def load_fg(e_, g, sl):
                            a1, a3, a2 = wsrc(e_)
                            a1v = a1.rearrange("(c p) f -> p c f", p=128)
                            a3v = a3.rearrange("(c p) f -> p c f", p=128)
                            a2v = a2.rearrange("(c p) d -> p c d", p=128)
                            S.dma("pool", f"ffw{sl}", [(w1b[sl][:], a1v[:, :, g * 256:(g + 1) * 256]),
                                                       (w3b[sl][:], a3v[:, :, g * 256:(g + 1) * 256]),
                                                       (w2b[sl][:], a2v[:, 2 * g:2 * g + 2, :])],
                                  writes=[("ffw", sl)])
                        seq_fg = [(e_, g) for e_ in experts for g in range(NFG)] if run("ffn") else []
                        if seq_fg:
                            load_fg(seq_fg[0][0], seq_fg[0][1], 0)
                        si = 0
                        for idx, (e_, g) in enumerate(seq_fg):
                            sl = idx % 2
                            if idx + 1 < len(seq_fg):
                                load_fg(seq_fg[idx + 1][0], seq_fg[idx + 1][1], (idx + 1) % 2)
                            if moe and g == 0:
                                cbe = cbt[e_ % 2]
                                for t in qtiles:
                                    t0, tn = TILES[t]
                                    b_ = bank("d")
                                    for q in range(tn // 128):
                                        tt = t0 // 128 + q
                                        d_ = dg[tt % 2]
                                        S.op("dve", lambda e: e.tensor_scalar(d_[:], ident, comb[:, tt, e_:e_ + 1], None, ALU.mult),
                                             reads=["comb", "const"], writes=[("dg", tt % 2)])
                                        S.op("pe", lambda e: e.matmul(ps[b_][:, q * 128:(q + 1) * 128], ones_1, d_[:], start=True, stop=True),
                                             reads=[("dg", tt % 2), "const"], writes=[psk[b_]])
                                    S.op("act", lambda e: e.activation(out=cbe[:, t0:t0 + tn], in_=ps[b_][:, :tn], func=AF.Identity), reads=[psk[b_]], writes=[("cbt", e_ % 2)])
                            ab = act_[sl]
                            for t in qtiles:
                                t0, tn = TILES[t]
                                for f in range(2):
                                    b1, b3 = bank("a"), bank("b")
                                    for k in range(8):
                                        S.op("pe", lambda e: e.matmul(ps[b1][:, :tn], w1b[sl][:, k, f * 128:(f + 1) * 128], u[:, k, t0:t0 + tn],
                                                                      start=(k == 0), stop=(k == 7)),
                                             reads=[("ffw", sl), uk(t)], writes=[psk[b1]])
                                    for k in range(8):
                                        S.op("pe", lambda e: e.matmul(ps[b3][:, :tn], w3b[sl][:, k, f * 128:(f + 1) * 128], u[:, k, t0:t0 + tn],
                                                                      start=(k == 0), stop=(k == 7)),
                                             reads=[("ffw", sl), uk(t)], writes=[psk[b3]])
                                    stt, sk = st_[si % 2], ("st", si % 2)
                                    S.op("act", lambda e: e.activation(out=stt[:, :tn], in_=ps[b1][:, :tn], func=AF.Silu), reads=[psk[b1]], writes=[sk])
                                    if moe:
                                        ptt, pk = pt_[si % 2], ("pt", si % 2)
                                        S.op("dve", lambda e: e.tensor_tensor(ptt[:, :tn], stt[:, :tn], ps[b3][:, :tn], ALU.mult),
                                             reads=[sk, psk[b3]], writes=[pk])
                                        S.op("dve", lambda e: e.tensor_tensor(ab[:, f, t0:t0 + tn], ptt[:, :tn], cbt[e_ % 2][:, t0:t0 + tn], ALU.mult),
                                             reads=[pk, ("cbt", e_ % 2)], writes=[("act", sl, t, f)])
                                    else:
                                        S.op("dve", lambda e: e.tensor_tensor(ab[:, f, t0:t0 + tn], stt[:, :tn], ps[b3][:, :tn], ALU.mult),
                                             reads=[sk, psk[b3]], writes=[("act", sl, t, f)])
                                    si += 1
                            for m in range(8):
                                for t in qtiles:
                                    t0, tn = TILES[t]
                                    n = tile_n(t, s)
                                    via_act = (dj % 8) in (1, 4, 6)
                                    dj += 1
                                    if via_act:
                                        b_ = bank("d")
                                    else:
                                        b_ = (4, 5, 0, 1, 2, 3)[dvr % 6]
                                        dvr += 1
                                    for f in range(2):
                                        S.op("pe", lambda e: e.matmul(ps[b_][:, :tn], w2b[sl][:, f, m * 128:(m + 1) * 128], ab[:, f, t0:t0 + tn],
                                                                      start=(f == 0), stop=(f == 1)),
                                             reads=[("ffw", sl), ("act", sl, t, f)], writes=[psk[b_]])
                                    if via_act:
                                        ev_, evk = evt[ej % 2], ("evt", ej % 2)
                                        ej += 1
                                        S.op("act", lambda e: e.activation(out=ev_[:, :tn], in_=ps[b_][:, :tn], func=AF.Identity, scale=M(l, 40 + m, n)),
                                             reads=[psk[b_], "mod"], writes=[evk])
                                        S.op("pool", lambda e: e.tensor_tensor(h[:, m, t0:t0 + tn], h[:, m, t0:t0 + tn], ev_[:, :tn], ALU.add),
                                             reads=[evk, hk(m, t)], writes=[hk(m, t)])
                                    else:
                                        S.op("dve", lambda e: e.scalar_tensor_tensor(h[:, m, t0:t0 + tn], ps[b_][:, :tn], M(l, 40 + m, n),
                                                                                     h[:, m, t0:t0 + tn], ALU.mult, ALU.add),
                                             reads=[psk[b_], hk(m, t)], writes=[hk(m, t)])
                        S.barrier()

                with ExitStack() as ph:
                    nl = (l + 1 < NL)
                    if run("ln2"):
                        layer_norm(l, s, 2, qtiles, ((l + 1, 8, 0) if nl else None), ph)
                    S.barrier()

            yv = yT[s].rearrange("(c p) t -> p c t", p=128)
            S.dma("sp", "yst", [(yv[:, 0:4, :], h[:, 0:4, CTX:]), (yv[:, 4:8, :], h[:, 4:8, CTX:])],
                  reads=[hk(c, t) for c in range(8) for t in range(5)], writes=["y"])
            S.final_wait("sp", "yst")
            S.barrier()
    return nc


def _rope_tables():
    rows = SEQ // 64
    row = np.repeat(np.arange(rows, dtype=np.float32), 64)
    col = np.tile(np.arange(64, dtype=np.float32), rows)
    inv = (10000.0 ** (-np.arange(16, dtype=np.float32) / 16)).astype(np.float32)
    ang = np.concatenate([row[:, None] * inv, col[:, None] * inv], -1)
    jj = (np.arange(128) % 64) // 2
    return np.ascontiguousarray(np.cos(ang)[:, jj].T.astype(np.float32)), np.ascontiguousarray(np.sin(ang)[:, jj].T.astype(np.float32))


def _host_layout(inp):
    f = lambda a: np.ascontiguousarray(np.asarray(a, dtype=np.float32))
    sh = {}
    sh["w_mod"] = f(inp["w_mod"])
    sh["b_modT"] = f(np.asarray(inp["b_mod"]).reshape(DEPTH, 48, 128).transpose(2, 0, 1))
    sh["w_in"] = f(inp["w_in"])
    sh["w_out"] = f(inp["w_out"])
    lam = np.stack([np.asarray(inp[k]) for k in ("lam_q1", "lam_k1", "lam_q2", "lam_k2")], 1)
    sh["lamqk"] = f(np.broadcast_to(lam[None], (128, DEPTH, 4, 64)))
    sh["sublnT"] = f(np.asarray(inp["subln_g"]).T)
    cw = np.asarray(inp["conv_w"]).reshape(DEPTH, 4, 4, 128)
    cbias = np.asarray(inp["conv_b"]).reshape(DEPTH, 1, 4, 128)
    sh["convT"] = f(np.concatenate([cw, cbias], 1).transpose(3, 0, 2, 1))
    rv = np.stack([np.asarray(inp[k]).reshape(DEPTH, 2, 4, 128) for k in ("rg_ba", "rg_bx", "rg_lambda")], -1)
    sh["rgvT"] = f(rv.transpose(3, 0, 1, 2, 4))
    gb = np.zeros((DEPTH, 2, 4, 2, 128, 128), np.float32)
    for g, k in enumerate(("rg_wa", "rg_wx")):
        w = np.asarray(inp[k])
        for j in range(4):
            gb[:, :, j, g, 0:64, 0:64] = w[:, :, 2 * j]
            gb[:, :, j, g, 64:128, 64:128] = w[:, :, 2 * j + 1]
    sh["gate_bd"] = gb
    ln = np.stack([np.asarray(inp[k]).reshape(DEPTH, 8, 128) for k in ("ln1_g", "ln1_b", "ln2_g", "ln2_b")], 1)
    sh["lnT"] = f(ln.transpose(3, 0, 1, 2))
    for k in ("ffn_w1", "ffn_w3", "ffn_w2", "moe_w1", "moe_w3", "moe_w2"):
        sh[k] = f(inp[k])
    sh["routerT"] = f(np.asarray(inp["moe_router"]).reshape(2, 8, 128, NE).transpose(2, 0, 1, 3))
    c, s_ = _rope_tables()
    sh["cosd"], sh["sind"] = c, s_
    cfm = np.zeros((128, 4, 128), np.float32)
    cfm[:, 0, :] = np.eye(128, dtype=np.float32)
    cfm[:, 1, :] = 1.0 / 1024
    cfm[:, 2, :] = 1.0 / 128
    cfm[:, 3, :] = 1.0
    sh["constf"] = cfm
    cbm = np.zeros((128, 2, 128), np.float32)
    cbm[:, 0, :] = 1.0
    for j in range(64):
        cbm[2 * j + 1, 1, 2 * j] = -1.0
        cbm[2 * j, 1, 2 * j + 1] = 1.0
    sh["constb"] = cbm.astype(ml_dtypes.bfloat16)
    return sh


def kernel(**inputs):
    NL, NSEQ, NC = CFG["NL"], CFG["NSEQ"], CFG["NCORES"]
    shared = _host_layout(inputs)
    x = np.asarray(inputs["x"], dtype=np.float32)
    ctx = np.asarray(inputs["ctx"], dtype=np.float32)
    c = np.asarray(inputs["c"], dtype=np.float32)
    c_ctx = np.asarray(inputs["c_ctx"], dtype=np.float32)
    in_maps = []
    for core in range(NC):
        bs = [core * NSEQ + i for i in range(NSEQ)]
        m = dict(shared)
        m["xT"] = np.ascontiguousarray(x[bs].transpose(0, 2, 1))
        m["ctxT"] = np.ascontiguousarray(ctx[bs].transpose(0, 2, 1))
        cols = [c[b] for b in bs]
        while len(cols) < 2:
            cols.append(cols[-1])
        cols.append(c_ctx)
        m["cT"] = np.ascontiguousarray(np.stack(cols, -1).reshape(8, 128, 3).transpose(1, 0, 2))
        in_maps.append(m)
    nc = build(NL, NSEQ)
    res = run_bass_kernel_spmd(nc, in_maps, core_ids=list(range(NC)))
    out = np.empty((NC * NSEQ, SEQ, D), np.float32)
    for core in range(NC):
        y = res.results[core]["yT"]
        for i in range(NSEQ):
            out[core * NSEQ + i] = y[i].T
    return out
```

```python
import math
from contextlib import ExitStack

import numpy as np
import ml_dtypes

import concourse.bass as bass
import concourse.mybir as mybir
from concourse.bass_utils import run_bass_kernel_spmd

F32 = mybir.dt.float32
BF16 = mybir.dt.bfloat16
ALU = mybir.AluOpType
AF = mybir.ActivationFunctionType
AX = mybir.AxisListType

D = 1024
DEPTH = 4
SEQ = 2048
CTX = 256
T = SEQ + CTX
IN_W = 2560
D_FF = 2816
NE = 8
ALPHA = (2 * DEPTH) ** 0.25
LN_EPS = 1e-5 / (ALPHA * ALPHA)
RMS_EPS = 1e-5
ATTN_SCALE = 64 ** -0.5
TILES = [(0, 256), (256, 512), (768, 512), (1280, 512), (1792, 512)]
NFG = D_FF // 256

CFG = {"NL": 4, "NSEQ": 2, "NCORES": 8, "STOP": "ln2"}
_ORDER = ["load", "rg", "attn", "ln1", "ffn", "ln2"]


def run(stage):
    return _ORDER.index(stage) <= _ORDER.index(CFG["STOP"])


class Sync:
    def __init__(self, nc, es):
        self.nc = nc
        self.eng = {"pe": nc.tensor, "act": nc.scalar, "dve": nc.vector, "pool": nc.gpsimd, "sp": nc.sync}
        self.sem, self.cnt, self.seen, self.state = {}, {}, {}, {}
        self.es = es
        for e in self.eng:
            self.newsem(e)

    def newsem(self, name):
        self.sem[name] = self.es.enter_context(self.nc.semaphore(name))
        self.cnt[name] = 0

    def _deps(self, me, reads, writes):
        deps = {}

        def add(p):
            if p is not None and deps.get(p[0], 0) < p[1]:
                deps[p[0]] = p[1]
        for k in reads:
            st = self.state.get(k)
            if st:
                add(st[0])
                if isinstance(k, tuple) and k[0] == "ps":
                    for s, v in st[1].items():
                        if s != me:
                            add((s, v))
        for k in writes:
            st = self.state.get(k)
            if st:
                add(st[0])
                for s, v in st[1].items():
                    add((s, v))
        return deps

    def _wait(self, me, deps):
        seen = self.seen.setdefault(me, {})
        for s, v in deps.items():
            if s == me and me == "pe":
                continue
            if seen.get(s, 0) >= v:
                continue
            self.eng[me].wait_ge(self.sem[s], v)
            seen[s] = v

    def _record(self, tag, reads, writes):
        for k in reads:
            st = self.state.setdefault(k, [None, {}])
            st[1][tag[0]] = tag[1]
        for k in writes:
            self.state[k] = [tag, {}]

    def op(self, me, fn, reads=(), writes=()):
        self._wait(me, self._deps(me, reads, writes))
        fn(self.eng[me]).then_inc(self.sem[me], 1)
        self.cnt[me] += 1
        self._record((me, self.cnt[me]), reads, writes)

    def dma(self, queue, semname, xfers, reads=(), writes=()):
        if semname not in self.sem:
            self.newsem(semname)
        self._wait(queue, self._deps(semname, reads, writes))
        for (o, i) in xfers:
            self.eng[queue].dma_start(out=o, in_=i).then_inc(self.sem[semname], 16)
            self.cnt[semname] += 16
        self._record((semname, self.cnt[semname]), reads, writes)

    def barrier(self):
        for me in self.eng:
            self._wait(me, {s: v for s, v in self.cnt.items() if v > 0 and s != me})

    def final_wait(self, me, semname):
        self._wait(me, {semname: self.cnt[semname]})


def build(NL, NSEQ):
    nc = bass.Bass("TRN2", target_bir_lowering=False)

    def din(name, shape, dt=F32):
        return nc.dram_tensor(name, list(shape), dt, kind="ExternalInput").ap()

    xT = din("xT", [NSEQ, D, SEQ])
    ctxT = din("ctxT", [NSEQ, D, CTX])
    cT = din("cT", [128, 8, 3])
    w_mod = din("w_mod", [DEPTH, D, 6 * D])
    b_modT = din("b_modT", [128, DEPTH, 48])
    w_in = din("w_in", [DEPTH, D, IN_W])
    w_out = din("w_out", [DEPTH, D, D])
    lamqk = din("lamqk", [128, DEPTH, 4, 64])
    sublnT = din("sublnT", [128, DEPTH])
    convT = din("convT", [128, DEPTH, 4, 5])
    rgvT = din("rgvT", [128, DEPTH, 2, 4, 3])
    gate_bd = din("gate_bd", [DEPTH, 2, 4, 2, 128, 128])
    lnT = din("lnT", [128, DEPTH, 4, 8])
    ffn_w1 = din("ffn_w1", [2, D, D_FF])
    ffn_w3 = din("ffn_w3", [2, D, D_FF])
    ffn_w2 = din("ffn_w2", [2, D_FF, D])
    routerT = din("routerT", [128, 2, 8, NE])
    moe_w1 = din("moe_w1", [2, NE, D, D_FF])
    moe_w3 = din("moe_w3", [2, NE, D, D_FF])
    moe_w2 = din("moe_w2", [2, NE, D_FF, D])
    cosd = din("cosd", [128, SEQ])
    sind = din("sind", [128, SEQ])
    constf = din("constf", [128, 4, 128])
    constb = din("constb", [128, 2, 128], BF16)
    yT = nc.dram_tensor("yT", [NSEQ, D, SEQ], F32, kind="ExternalOutput").ap()

    es = ExitStack()
    with es:
        S = Sync(nc, es)

        uid = [0]

        def sb(scope, name, shape, dt=F32):
            uid[0] += 1
            return scope.enter_context(nc.sbuf_tensor(f"{name}_{uid[0]}", list(shape), dt))

        ps = [es.enter_context(nc.psum_tensor(f"ps{i}", [128, 512], F32)) for i in range(8)]
        psk = [("ps", i) for i in range(8)]
        rr = {"a": 0, "b": 0, "c": 0, "d": 0}

        def bank(role):
            base = {"a": 0, "b": 2, "c": 4, "d": 6}[role]
            i = base + rr[role] % 2
            rr[role] += 1
            return i

        h = sb(es, "h", [128, 8, T])
        u = sb(es, "u", [128, 8, T], BF16)
        cos = sb(es, "cos", [128, SEQ])
        sin = sb(es, "sin", [128, SEQ])
        cf = sb(es, "cf", [128, 4, 128])
        cb16 = sb(es, "cb16", [128, 2, 128], BF16)
        mod = sb(es, "mod", [128, DEPTH, 48, 3])
        lnp = sb(es, "lnp", [128, DEPTH, 4, 8])
        convp = sb(es, "convp", [128, DEPTH, 4, 5])
        rgv = sb(es, "rgv", [128, DEPTH, 2, 4, 3])
        rgc = sb(es, "rgc", [128, DEPTH, 2, 4])
        subg = sb(es, "subg", [128, DEPTH])
        lamv = sb(es, "lamv", [128, DEPTH, 4])
        rout = sb(es, "rout", [128, 2, 8, NE])
        ident = cf[:, 0, :]
        ones_d = cf[:, 1, :]
        ones_h = cf[:, 2, :]
        ones_1 = cf[:, 3, :]
        onesb = cb16[:, 0, :]
        Rb = cb16[:, 1, :]

        S.dma("sp", "const", [(cos[:], cosd[:, :]), (sin[:], sind[:, :]), (cf[:], constf[:, :, :]),
                              (cb16[:], constb[:, :, :]), (lnp[:], lnT[:, :, :, :]), (convp[:], convT[:, :, :, :]),
                              (rgv[:], rgvT[:, :, :, :, :]), (subg[:], sublnT[:, :]),
                              (rout[:], routerT[:, :, :, :])],
              writes=["const"])

        def M(l, q, n):
            return mod[:, l, q, n:n + 1]

        def load_h(s):
            xv = xT[s].rearrange("(c p) t -> p c t", p=128)
            cv = ctxT[s].rearrange("(c p) t -> p c t", p=128)
            S.dma("pool", "hld", [(h[:, 0:4, CTX:], xv[:, 0:4, :]), (h[:, 4:8, CTX:], xv[:, 4:8, :]), (h[:, :, 0:CTX], cv[:, :, :])],
                  writes=[("h", c, t) for c in range(8) for t in range(5)])
        load_h(0)

        with ExitStack() as ph:
            scT = sb(ph, "scT", [128, 8, 3])
            bmod = sb(ph, "bmod", [128, DEPTH, 48])
            wm = [sb(ph, f"wm{i}", [128, 8, 512]) for i in range(2)]
            lamt = sb(ph, "lamt", [128, DEPTH, 4, 64])
            S.dma("sp", "cld", [(scT[:], cT[:, :, :]), (bmod[:], b_modT[:, :, :]), (lamt[:], lamqk[:, :, :, :])], writes=["scT", "bmod", "lamt"])
            S.op("act", lambda e: e.activation(out=scT[:], in_=scT[:], func=AF.Silu), reads=["scT"], writes=["scT"])
            it = 0
            for l in range(NL):
                wv_ = w_mod[l].rearrange("(c p) f -> p c f", p=128)
                for j in range(12):
                    sl = it % 2
                    it += 1
                    S.dma("sp", f"wm{sl}", [(wm[sl][:, 0:4, :], wv_[:, 0:4, j * 512:(j + 1) * 512]),
                                            (wm[sl][:, 4:8, :], wv_[:, 4:8, j * 512:(j + 1) * 512])],
                          writes=[("wm", sl)])
                    for fc in range(4):
                        b_ = bank("d")
                        for k in range(8):
                            S.op("pe", lambda e: e.matmul(ps[b_][:, 0:3], wm[sl][:, k, fc * 128:(fc + 1) * 128], scT[:, k, :],
                                                          start=(k == 0), stop=(k == 7)),
                                 reads=[("wm", sl), "scT"], writes=[psk[b_]])
                        q = j * 4 + fc
                        S.op("dve", lambda e: e.tensor_scalar(mod[:, l, q, :], ps[b_][:, 0:3], bmod[:, l, q:q + 1], None, ALU.add),
                             reads=[psk[b_], "bmod"], writes=["mod"])
                for g0 in (8, 32):
                    S.op("dve", lambda e: e.tensor_scalar(mod[:, l, g0:g0 + 8, :], mod[:, l, g0:g0 + 8, :], 1.0, None, ALU.add),
                         reads=["mod"], writes=["mod"])
                for g0 in (16, 40):
                    S.op("dve", lambda e: e.tensor_scalar(mod[:, l, g0:g0 + 8, :], mod[:, l, g0:g0 + 8, :], 1.0, 1.0 / ALPHA,
                                                          ALU.add, ALU.mult),
                         reads=["mod"], writes=["mod"])
            S.op("act", lambda e: e.activation(out=rgc[:], in_=rgv[:, :, :, :, 2], func=AF.Exp, scale=-1.0),
                 reads=["const"], writes=["rgc"])
            S.op("dve", lambda e: e.tensor_scalar(rgc[:], rgc[:], 1.0, None, ALU.add), reads=["rgc"], writes=["rgc"])
            S.op("act", lambda e: e.activation(out=rgc[:], in_=rgc[:], func=AF.Ln), reads=["rgc"], writes=["rgc"])
            S.op("dve", lambda e: e.tensor_scalar(rgc[:], rgc[:], -8.0, None, ALU.mult), reads=["rgc"], writes=["rgc"])
            for l in range(NL):
                lam_init = 0.8 - 0.6 * math.exp(-0.3 * l)
                S.op("dve", lambda e: e.tensor_tensor(lamt[:, l, 0, :], lamt[:, l, 0, :], lamt[:, l, 1, :], ALU.mult),
                     reads=["lamt"], writes=["lamt"])
                S.op("dve", lambda e: e.tensor_tensor(lamt[:, l, 2, :], lamt[:, l, 2, :], lamt[:, l, 3, :], ALU.mult),
                     reads=["lamt"], writes=["lamt"])
                S.op("dve", lambda e: e.reduce_sum(lamv[:, l, 0:1], lamt[:, l, 0, :], AX.X), reads=["lamt"], writes=["lamv"])
                S.op("dve", lambda e: e.reduce_sum(lamv[:, l, 1:2], lamt[:, l, 2, :], AX.X), reads=["lamv", "lamt"], writes=["lamv"])
                S.op("act", lambda e: e.activation(out=lamv[:, l, 0:2], in_=lamv[:, l, 0:2], func=AF.Exp),
                     reads=["lamv"], writes=["lamv"])
                S.op("dve", lambda e: e.scalar_tensor_tensor(lamv[:, l, 2:3], lamv[:, l, 1:2], -lam_init, lamv[:, l, 0:1],
                                                             ALU.add, ALU.subtract),
                     reads=["lamv"], writes=["lamv"])
                S.op("dve", lambda e: e.tensor_scalar(subg[:, l:l + 1], subg[:, l:l + 1], 1.0 - lam_init, None, ALU.mult),
                     reads=["const", "lamv"], writes=["subg"])
            S.barrier()

        def tile_n(t, s):
            return 2 if t == 0 else s

        def hk(c, t):
            return ("h", c, t)

        def uk(t):
            return ("u", t)

        def out_proj_partial(l, s, t, wo, wokey, mixt, mixkey, role="c", evs=None):
            t0, tn = TILES[t]
            n = tile_n(t, s)
            for m in range(8):
                if evs is not None and m % 3 == 1:
                    b_ = bank("d")
                    S.op("pe", lambda e: e.matmul(ps[b_][:, :tn], wo[:, m * 128:(m + 1) * 128], mixt[:, :tn], start=True, stop=True),
                         reads=[wokey, mixkey], writes=[psk[b_]])
                    ev_, evk = evs[m % 2], ("oevt", m % 2)
                    S.op("act", lambda e: e.activation(out=ev_[:, :tn], in_=ps[b_][:, :tn], func=AF.Identity, scale=M(l, 16 + m, n)),
                         reads=[psk[b_], "mod"], writes=[evk])
                    S.op("pool", lambda e: e.tensor_tensor(h[:, m, t0:t0 + tn], h[:, m, t0:t0 + tn], ev_[:, :tn], ALU.add),
                         reads=[evk, hk(m, t)], writes=[hk(m, t)])
                    continue
                b_ = bank(role)
                S.op("pe", lambda e: e.matmul(ps[b_][:, :tn], wo[:, m * 128:(m + 1) * 128], mixt[:, :tn], start=True, stop=True),
                     reads=[wokey, mixkey], writes=[psk[b_]])
                S.op("dve", lambda e: e.scalar_tensor_tensor(h[:, m, t0:t0 + tn], ps[b_][:, :tn], M(l, 16 + m, n),
                                                             h[:, m, t0:t0 + tn], ALU.mult, ALU.add),
                     reads=[psk[b_], hk(m, t)], writes=[hk(m, t)])

        def layer_norm(l, s, which, tiles, nxt, ph, router_j=None, lg=None):
            gq, bq = (0, 1) if which == 1 else (2, 3)
            tmp = [sb(ph, f"ln_tmp{i}", [128, 512]) for i in range(2)]
            msb = sb(ph, "ln_m", [128, 512])
            rstd = sb(ph, "ln_r", [128, 512])
            sq = [sb(ph, f"ln_sq{i}", [128, 512]) for i in range(2)]
            u32 = [sb(ph, f"ln_u32{i}", [128, 512]) for i in range(2)] if router_j is not None else None
            lgT = sb(ph, "ln_lgT", [8, 512]) if router_j is not None else None
            for t in tiles:
                t0, tn = TILES[t]
                n = tile_n(t, s)
                bm, bv = bank("d"), bank("d")
                for c in range(8):
                    S.op("pe", lambda e: e.matmul(ps[bm][:, :tn], ones_d, h[:, c, t0:t0 + tn], start=(c == 0), stop=(c == 7)),
                         reads=[hk(c, t), "const"], writes=[psk[bm]])
                for c in range(8):
                    sq_ = sq[c % 2]
                    S.op("act", lambda e: e.activation(out=sq_[:, :tn], in_=h[:, c, t0:t0 + tn], func=AF.Square),
                         reads=[hk(c, t)], writes=[("lnsq", c % 2)])
                    S.op("pe", lambda e: e.matmul(ps[bv][:, :tn], ones_d, sq_[:, :tn], start=(c == 0), stop=(c == 7)),
                         reads=[("lnsq", c % 2), "const"], writes=[psk[bv]])
                S.op("act", lambda e: e.activation(out=msb[:, :tn], in_=ps[bm][:, :tn], func=AF.Identity), reads=[psk[bm]], writes=["lnm"])
                S.op("dve", lambda e: e.tensor_tensor(rstd[:, :tn], msb[:, :tn], msb[:, :tn], ALU.mult), reads=["lnm"], writes=["lnr"])
                S.op("dve", lambda e: e.scalar_tensor_tensor(rstd[:, :tn], ps[bv][:, :tn], LN_EPS, rstd[:, :tn], ALU.add, ALU.subtract),
                     reads=[psk[bv], "lnr"], writes=["lnr"])
                S.op("act", lambda e: e.activation(out=rstd[:, :tn], in_=rstd[:, :tn], func=AF.Ln), reads=["lnr"], writes=["lnr"])
                S.op("act", lambda e: e.activation(out=rstd[:, :tn], in_=rstd[:, :tn], func=AF.Exp, scale=-0.5), reads=["lnr"], writes=["lnr"])
                if router_j is not None:
                    bl = bank("c")
                for c in range(8):
                    tm = tmp[c % 2]
                    hv = h[:, c, t0:t0 + tn]
                    S.op("dve", lambda e: e.tensor_tensor(tm[:, :tn], hv, msb[:, :tn], ALU.subtract),
                         reads=[hk(c, t), "lnm"], writes=[("lntmp", c % 2)])
                    S.op("dve", lambda e: e.tensor_tensor(tm[:, :tn], tm[:, :tn], rstd[:, :tn], ALU.mult),
                         reads=[("lntmp", c % 2), "lnr"], writes=[("lntmp", c % 2)])
                    S.op("dve", lambda e: e.tensor_scalar(hv, tm[:, :tn], lnp[:, l, gq, c:c + 1], lnp[:, l, bq, c:c + 1], ALU.mult, ALU.add),
                         reads=[("lntmp", c % 2), "const"], writes=[hk(c, t)])
                    if nxt is not None:
                        l2, oq, sq0 = nxt
                        S.op("act", lambda e: e.activation(out=u[:, c, t0:t0 + tn], in_=hv, func=AF.Identity,
                                                           bias=M(l2, sq0 + c, n), scale=M(l2, oq + c, n)),
                             reads=[hk(c, t), "mod"], writes=[uk(t)])
                        if router_j is not None:
                            uu = u32[c % 2]
                            S.op("dve", lambda e: e.tensor_scalar(uu[:, :tn], hv, M(l2, oq + c, n), M(l2, sq0 + c, n), ALU.mult, ALU.add),
                                 reads=[hk(c, t), "mod"], writes=[("u32", c % 2)])
                            S.op("pe", lambda e: e.matmul(ps[bl][0:8, :tn], rout[:, router_j, c, :], uu[:, :tn], start=(c == 0), stop=(c == 7)),
                                 reads=[("u32", c % 2), "const"], writes=[psk[bl]])
                if router_j is not None:
                    S.op("act", lambda e: e.activation(out=lgT[:, :tn], in_=ps[bl][0:8, :tn], func=AF.Identity), reads=[psk[bl]], writes=["lgT"])
                    for q in range(tn // 128):
                        b2 = bank("d")
                        S.op("pe", lambda e: e.matmul(ps[b2][:, 0:8], lgT[:, q * 128:(q + 1) * 128], ident[0:8, 0:8], start=True, stop=True),
                             reads=["lgT", "const"], writes=[psk[b2]])
                        tt = t0 // 128 + q
                        S.op("dve", lambda e: e.tensor_copy(lg[:, tt, :], ps[b2][:, 0:8]), reads=[psk[b2]], writes=["lg"])

        for s in range(NSEQ):
            if s > 0:
                load_h(s)
            for t in range(5):
                t0, tn = TILES[t]
                n = tile_n(t, s)
                for c in range(8):
                    S.op("dve", lambda e: e.tensor_scalar(u[:, c, t0:t0 + tn], h[:, c, t0:t0 + tn], M(0, 8 + c, n), M(0, c, n), ALU.mult, ALU.add),
                         reads=[hk(c, t), "mod"], writes=[uk(t)])

            for l in range(NL):
                last = (l == DEPTH - 1)
                qtiles = [1, 2, 3, 4] if last else [0, 1, 2, 3, 4]
                win = w_in[l].rearrange("(c p) f -> p c f", p=128)
                wout = w_out[l]

                with ExitStack() as ph:
                    xrp = sb(ph, "xrp", [128, T + 8])
                    xc = sb(ph, "xc", [128, T])
                    A = sb(ph, "A", [128, T])
                    Bm = sb(ph, "Bm", [128, T])
                    H = sb(ph, "H", [128, T])
                    wxr = [sb(ph, f"wxr{i}", [128, 8, 128], BF16) for i in range(2)]
                    wy = [sb(ph, f"wy{i}", [128, 8, 128], BF16) for i in range(2)]
                    wo = [sb(ph, f"wo{i}", [128, D], BF16) for i in range(2)]
                    wg = [sb(ph, f"wg{i}", [128, 4, 128]) for i in range(2)]
                    tmpf = [sb(ph, f"rtmp{i}", [128, 512]) for i in range(2)]
                    oev = [sb(ph, f"oev{i}", [128, 512]) for i in range(2)]
                    mixt = [sb(ph, f"rmix{i}", [128, 512], BF16) for i in range(2)]
                    mi = 0
                    CO, LO = 2, 261

                    def load_rg(j):
                        sl = j % 2
                        S.dma("pool", f"rgwp{sl}", [(wxr[sl][:], win[:, :, 1024 + j * 128:1024 + (j + 1) * 128]),
                                                    (wy[sl][:], win[:, :, 2048 + j * 128:2048 + (j + 1) * 128]),
                                                    (wo[sl][:], wout[(4 + j) * 128:(5 + j) * 128, :])],
                              writes=[("rgwp", sl)])
                        S.dma("sp", f"rgws{sl}", [(wg[sl][:, d * 2 + g, :], gate_bd[l, d, j, g]) for d in range(2) for g in range(2)],
                              writes=[("rgws", sl)])
                    if run("rg"):
                        load_rg(0)
                    for j in range(4 if run("rg") else 0):
                        sl = j % 2
                        if j + 1 < 4:
                            load_rg(j + 1)
                        S.op("pool", lambda e: e.memset(xrp[:, 0:2], 0.0), writes=["xrp"])
                        S.op("pool", lambda e: e.memset(xrp[:, 258:261], 0.0), reads=["xrp"], writes=["xrp"])
                        S.op("pool", lambda e: e.memset(xrp[:, 2309:2312], 0.0), reads=["xrp"], writes=["xrp"])
                        for t in range(5):
                            t0, tn = TILES[t]
                            b_ = bank("a")
                            for k in range(8):
                                S.op("pe", lambda e: e.matmul(ps[b_][:, :tn], wxr[sl][:, k, :], u[:, k, t0:t0 + tn], start=(k == 0), stop=(k == 7)),
                                     reads=[("rgwp", sl), uk(t)], writes=[psk[b_]])
                            o0 = CO if t == 0 else LO + (t0 - CTX)
                            S.op("act", lambda e: e.activation(out=xrp[:, o0:o0 + tn], in_=ps[b_][:, :tn], func=AF.Identity), reads=[psk[b_], "xrp"], writes=["xrp"])
                        for (dst0, n_, src0) in ((0, CTX, CO - 2), (CTX, SEQ, LO - 2)):
                            S.op("dve", lambda e: e.tensor_scalar(xc[:, dst0:dst0 + n_], xrp[:, src0:src0 + n_], convp[:, l, j, 0:1],
                                                                  convp[:, l, j, 4:5], ALU.mult, ALU.add),
                                 reads=["xrp", "const"], writes=["xc"])
                            for tap in range(1, 4):
                                S.op("dve", lambda e: e.scalar_tensor_tensor(xc[:, dst0:dst0 + n_], xrp[:, src0 + tap:src0 + tap + n_],
                                                                             convp[:, l, j, tap:tap + 1], xc[:, dst0:dst0 + n_], ALU.mult, ALU.add),
                                     reads=["xrp", "xc"], writes=["xc"])
                        for d in range(2):
                            for t in range(5):
                                t0, tn = TILES[t]
                                for g, dst, key in ((0, A, "A"), (1, Bm, "B")):
                                    b_ = bank("b")
                                    S.op("pe", lambda e: e.matmul(ps[b_][:, :tn], wg[sl][:, d * 2 + g, :], xc[:, t0:t0 + tn], start=True, stop=True),
                                         reads=[("rgws", sl), "xc"], writes=[psk[b_]])
                                    S.op("act", lambda e: e.activation(out=dst[:, t0:t0 + tn], in_=ps[b_][:, :tn], func=AF.Sigmoid,
                                                                       bias=rgv[:, l, d, j, g:g + 1]),
                                         reads=[psk[b_], "const"], writes=[key])
                            S.op("act", lambda e: e.activation(out=A[:], in_=A[:], func=AF.Exp, scale=rgc[:, l, d, j:j + 1]),
                                 reads=["A", "rgc"], writes=["A"])
                            S.op("dve", lambda e: e.tensor_tensor(Bm[:], Bm[:], xc[:], ALU.mult), reads=["B", "xc"], writes=["B"])
                            scr = xrp[:, 0:T]
                            S.op("act", lambda e: e.activation(out=scr, in_=A[:], func=AF.Square), reads=["A", "xrp"], writes=["xrp"])
                            S.op("dve", lambda e: e.tensor_scalar(scr, scr, -1.0, 1.0, ALU.mult, ALU.add), reads=["xrp"], writes=["xrp"])
                            S.op("dve", lambda e: e.tensor_scalar(scr, scr, 0.0, None, ALU.max), reads=["xrp"], writes=["xrp"])
                            S.op("act", lambda e: e.activation(out=scr, in_=scr, func=AF.Sqrt), reads=["xrp"], writes=["xrp"])
                            S.op("dve", lambda e: e.tensor_tensor(Bm[:], Bm[:], scr, ALU.mult), reads=["B", "xrp"], writes=["B"])
                            if d == 0:
                                S.op("dve", lambda e: e.tensor_tensor_scan(H[:, 0:CTX], A[:, 0:CTX], Bm[:, 0:CTX], 0.0, ALU.mult, ALU.add),
                                     reads=["A", "B"], writes=["H"])
                                S.op("dve", lambda e: e.tensor_tensor_scan(H[:, CTX:], A[:, CTX:], Bm[:, CTX:], H[:, CTX - 1:CTX], ALU.mult, ALU.add),
                                     reads=["A", "B", "H"], writes=["H"])
                            else:
                                C = xrp
                                S.op("dve", lambda e: e.tensor_tensor_scan(C[:, 0:CTX][:, ::-1], A[:, 0:CTX][:, ::-1], Bm[:, 0:CTX][:, ::-1],
                                                                           0.0, ALU.mult, ALU.add),
                                     reads=["A", "B", "xrp"], writes=["xrp"])
                                S.op("dve", lambda e: e.tensor_tensor_scan(C[:, CTX:T][:, ::-1], A[:, CTX:][:, ::-1], Bm[:, CTX:][:, ::-1],
                                                                           C[:, 0:1], ALU.mult, ALU.add),
                                     reads=["A", "B", "xrp"], writes=["xrp"])
                                S.op("dve", lambda e: e.tensor_tensor(H[:], H[:], C[:, 0:T], ALU.add), reads=["H", "xrp"], writes=["H"])
                        for t in qtiles:
                            t0, tn = TILES[t]
                            b_ = bank("a")
                            for k in range(8):
                                S.op("pe", lambda e: e.matmul(ps[b_][:, :tn], wy[sl][:, k, :], u[:, k, t0:t0 + tn], start=(k == 0), stop=(k == 7)),
                                     reads=[("rgwp", sl), uk(t)], writes=[psk[b_]])
                            tf, mx = tmpf[mi % 2], mixt[mi % 2]
                            kf, km = ("rtmp", mi % 2), ("rmix", mi % 2)
                            mi += 1
                            S.op("act", lambda e: e.activation(out=tf[:, :tn], in_=ps[b_][:, :tn], func=AF.Gelu_apprx_tanh),
                                 reads=[psk[b_]], writes=[kf])
                            S.op("dve", lambda e: e.tensor_tensor(mx[:, :tn], tf[:, :tn], H[:, t0:t0 + tn], ALU.mult), reads=[kf, "H"], writes=[km])
                            out_proj_partial(l, s, t, wo[sl], ("rgwp", sl), mx, km, evs=oev)
                    S.barrier()

                with ExitStack() as ph:
                    v = sb(ph, "v", [128, 18, 512], BF16)
                    wv = sb(ph, "wv", [128, 8, 512], BF16)
                    kh = sb(ph, "kh", [128, T], BF16)
                    qz = [sb(ph, f"qz{i}", [128, T], BF16) for i in range(2)]
                    wk = [sb(ph, f"wk{i}", [128, 8, 128], BF16) for i in range(2)]
                    wq = [sb(ph, f"wq{i}", [128, 8, 128], BF16) for i in range(2)]
                    wo = [sb(ph, f"awo{i}", [128, D], BF16) for i in range(2)]
                    E = [sb(ph, f"E{i}", [128, 512], BF16) for i in range(3)]
                    qb = [sb(ph, f"qb{i}", [128, 512], BF16) for i in range(2)]
                    rm = [sb(ph, f"rm{i}", [128, 512]) for i in range(2)]
                    at = sb(ph, "at", [128, 512])
                    sqt = sb(ph, "sqt", [128, 512])
                    rs = sb(ph, "rs", [128, 512])
                    r1t, r2t = [at], [sqt]
                    mixt = [sb(ph, f"amix{i}", [128, 512], BF16) for i in range(2)]
                    if run("attn"):
                        S.op("pool", lambda e: e.memset(qz[0][64:128, :], 0.0), writes=["qh"])
                        S.op("pool", lambda e: e.memset(qz[1][0:64, :], 0.0), writes=["qh"])

                    def load_head(hd):
                        sl = hd % 2
                        S.dma("pool", f"attw{sl}", [(wk[sl][:], win[:, :, hd * 128:(hd + 1) * 128]),
                                                    (wq[sl][:], win[:, :, 1536 + hd * 128:1536 + (hd + 1) * 128]),
                                                    (wo[sl][:], wout[hd * 128:(hd + 1) * 128, :])],
                              writes=[("attw", sl)])
                    if run("attn"):
                        S.dma("pool", "wv", [(wv[:], win[:, :, 512:1024])], writes=["wv"])
                        load_head(0)
                    for tt in range(18 if (run("attn") and CFG.get("ASUB", 9) >= 1) else 0):
                        b_ = bank("a")
                        tile_of = 0 if tt < 2 else 1 + (tt - 2) // 4
                        for k in range(8):
                            S.op("pe", lambda e: e.matmul(ps[b_][:, :], u[:, k, tt * 128:(tt + 1) * 128], wv[:, k, :], start=(k == 0), stop=(k == 7)),
                                 reads=["wv", uk(tile_of)], writes=[psk[b_]])
                        S.op("act", lambda e: e.activation(out=v[:, tt, :], in_=ps[b_][:, :], func=AF.Identity), reads=[psk[b_]], writes=["v"])
                    ei = 0
                    qi = 0
                    mi = 0
                    ASUB = CFG.get("ASUB", 9)
                    for hd in range(4 if (run("attn") and ASUB >= 2) else 0):
                        sl = hd % 2
                        if hd + 1 < 4:
                            load_head(hd + 1)

                        def proj_rope(wt, dst, dkey, tiles):
                            nonlocal qi
                            for t in tiles:
                                t0, tn = TILES[t]
                                b_ = bank("a")
                                for k in range(8):
                                    S.op("pe", lambda e: e.matmul(ps[b_][:, :tn], wt[:, k, :], u[:, k, t0:t0 + tn], start=(k == 0), stop=(k == 7)),
                                         reads=[("attw", sl), uk(t)], writes=[psk[b_]])
                                if t == 0:
                                    if isinstance(dst, list):
                                        S.op("act", lambda e: e.activation(out=dst[0][0:64, t0:t0 + tn], in_=ps[b_][0:64, :tn], func=AF.Identity), reads=[psk[b_]], writes=[dkey])
                                        S.op("act", lambda e: e.activation(out=dst[1][64:128, t0:t0 + tn], in_=ps[b_][64:128, :tn], func=AF.Identity), reads=[psk[b_]], writes=[dkey])
                                    else:
                                        S.op("act", lambda e: e.activation(out=dst[:, t0:t0 + tn], in_=ps[b_][:, :tn], func=AF.Identity), reads=[psk[b_]], writes=[dkey])
                                    continue
                                i2 = qi % 2
                                qi += 1
                                l0 = t0 - CTX
                                S.op("act", lambda e: e.activation(out=qb[i2][:, :tn], in_=ps[b_][:, :tn], func=AF.Identity), reads=[psk[b_]], writes=[("qb", i2)])
                                b2 = bank("b")
                                S.op("pe", lambda e: e.matmul(ps[b2][:, :tn], Rb, qb[i2][:, :tn], start=True, stop=True),
                                     reads=[("qb", i2), "const"], writes=[psk[b2]])
                                RSUB = CFG.get("RSUB", 9)
                                if RSUB < 2:
                                    continue
                                S.op("dve", lambda e: e.tensor_tensor(r1t[0][:, :tn], cos[:, l0:l0 + tn], ps[b_][:, :tn], ALU.mult),
                                     reads=[psk[b_], "const", ("qb", i2)], writes=["at"])
                                if RSUB == 2:
                                    continue
                                S.op("dve", lambda e: e.tensor_tensor(r2t[0][:, :tn], sin[:, l0:l0 + tn], ps[b2][:, :tn], ALU.mult),
                                     reads=[psk[b2], "const"], writes=["sqt"])
                                if RSUB < 3:
                                    continue
                                if isinstance(dst, list):
                                    S.op("dve", lambda e: e.tensor_tensor(dst[0][0:64, t0:t0 + tn], r1t[0][0:64, :tn], r2t[0][0:64, :tn], ALU.add),
                                         reads=["at", "sqt"], writes=[dkey])
                                    S.op("dve", lambda e: e.tensor_tensor(dst[1][64:128, t0:t0 + tn], r1t[0][64:128, :tn], r2t[0][64:128, :tn], ALU.add),
                                         reads=["at", "sqt"], writes=[dkey])
                                else:
                                    S.op("dve", lambda e: e.tensor_tensor(dst[:, t0:t0 + tn], r1t[0][:, :tn], r2t[0][:, :tn], ALU.add),
                                         reads=["at", "sqt"], writes=[dkey])
                        proj_rope(wk[sl], kh, "kh", [0, 1, 2, 3, 4] if ASUB >= 3 else [0])
                        if ASUB >= 4:
                            proj_rope(wq[sl], qz, "qh", qtiles)
                        LA = 2
                        steps = []
                        for t in (qtiles if ASUB >= 5 else []):
                            kts = [0, 1] if t == 0 else list(range(18))
                            for mp in range(2):
                                for ki, kt in enumerate(kts):
                                    steps.append((t, mp, ki, kt, len(kts)))
                        bo, bd = 4, 5
                        info = {}

                        def s_exp(i):
                            t, mp, ki, kt, nk = steps[i]
                            t0, tn = TILES[t]
                            b_ = i % 4
                            S.op("pe", lambda e: e.matmul(ps[b_][:, :tn], kh[:, kt * 128:(kt + 1) * 128], qz[mp][:, t0:t0 + tn],
                                                          start=True, stop=True),
                                 reads=["kh", "qh"], writes=[psk[b_]])
                            Et, ek = E[i % 3], ("E", i % 3)
                            S.op("act", lambda e: e.activation(out=Et[:, :tn], in_=ps[b_][:, :tn], func=AF.Exp, scale=ATTN_SCALE),
                                 reads=[psk[b_]], writes=[ek])

                        deferred = {}

                        def defer(step, fn):
                            deferred.setdefault(step, []).append(fn)

                        def pv_d(i):
                            nonlocal mi
                            t, mp, ki, kt, nk = steps[i]
                            t0, tn = TILES[t]
                            Et, ek = E[i % 3], ("E", i % 3)
                            S.op("pe", lambda e: e.matmul(ps[bo][:, :tn], v[:, kt, hd * 128:(hd + 1) * 128], Et[:, :tn],
                                                          start=(ki == 0), stop=(ki == nk - 1)),
                                 reads=[ek, "v"], writes=[psk[bo]])
                            S.op("pe", lambda e: e.matmul(ps[bd][:, :tn], onesb, Et[:, :tn],
                                                          start=(ki == 0), stop=(ki == nk - 1)),
                                 reads=[ek, "const"], writes=[psk[bd]])
                            if ki != nk - 1:
                                return
                            S.op("act", lambda e: e.activation(out=rs[:, :tn], in_=ps[bd][:, :tn], func=AF.Ln), reads=[psk[bd]], writes=["rs"])
                            S.op("act", lambda e: e.activation(out=rs[:, :tn], in_=rs[:, :tn], func=AF.Exp, scale=-1.0), reads=["rs"], writes=["rs"])
                            S.op("dve", lambda e: e.tensor_tensor(rm[mp][:, :tn], ps[bo][:, :tn], rs[:, :tn], ALU.mult),
                                 reads=[psk[bo], "rs"], writes=[("rm", mp)])
                            if mp == 0:
                                return
                            S.op("dve", lambda e: e.scalar_tensor_tensor(at[:, :tn], rm[1][:, :tn], lamv[:, l, 2:3], rm[0][:, :tn], ALU.mult, ALU.add),
                                 reads=[("rm", 0), ("rm", 1), "lamv"], writes=["at"])
                            S.op("act", lambda e: e.activation(out=sqt[:, :tn], in_=at[:, :tn], func=AF.Square), reads=["at"], writes=["sqt"])
                            mx, km = mixt[mi % 2], ("amix", mi % 2)
                            mi += 1
                            n = tile_n(t, s)

                            def stage2():
                                b_ = bank("d")
                                S.op("pe", lambda e: e.matmul(ps[b_][:, :tn], ones_h, sqt[:, :tn], start=True, stop=True),
                                     reads=["sqt", "const"], writes=[psk[b_]])
                                S.op("dve", lambda e: e.tensor_scalar(sqt[:, :tn], ps[b_][:, :tn], RMS_EPS, None, ALU.add), reads=[psk[b_], "sqt"], writes=["sqt"])
                                S.op("act", lambda e: e.activation(out=sqt[:, :tn], in_=sqt[:, :tn], func=AF.Ln), reads=["sqt"], writes=["sqt"])
                                S.op("act", lambda e: e.activation(out=sqt[:, :tn], in_=sqt[:, :tn], func=AF.Exp, scale=-0.5), reads=["sqt"], writes=["sqt"])
                                S.op("dve", lambda e: e.scalar_tensor_tensor(mx[:, :tn], at[:, :tn], subg[:, l:l + 1], sqt[:, :tn], ALU.mult, ALU.mult),
                                     reads=["at", "sqt", "subg"], writes=[km])

                            def stage3(m):
                                def f():
                                    b_ = bank("d")
                                    S.op("pe", lambda e: e.matmul(ps[b_][:, :tn], wo[sl][:, m * 128:(m + 1) * 128], mx[:, :tn], start=True, stop=True),
                                         reads=[("attw", sl), km], writes=[psk[b_]])
                                    S.op("dve", lambda e: e.scalar_tensor_tensor(h[:, m, t0:t0 + tn], ps[b_][:, :tn], M(l, 16 + m, n),
                                                                                 h[:, m, t0:t0 + tn], ALU.mult, ALU.add),
                                         reads=[psk[b_], hk(m, t)], writes=[hk(m, t)])
                                return f
                            cur = i + LA
                            defer(cur + 6, stage2)
                            for m in range(8):
                                defer(cur + 13 + m, stage3(m))
                        nst = len(steps)
                        i = 0
                        while i < nst + LA or any(k >= i for k in deferred):
                            if i < nst:
                                s_exp(i)
                            if 0 <= i - LA < nst:
                                pv_d(i - LA)
                            for fn in deferred.pop(i, []):
                                fn()
                            i += 1
                    S.barrier()

                moe = (l % 2 == 1)
                jj = l // 2
                with ExitStack() as ph2:
                    lg = sb(ph2, "lg", [128, 18, NE]) if moe else None
                    comb = sb(ph2, "comb", [128, 18, NE]) if moe else None
                    with ExitStack() as ph:
                        if run("ln1"):
                            layer_norm(l, s, 1, qtiles, (l, 32, 24), ph, router_j=(jj if moe else None), lg=lg)
                        S.barrier()

                    with ExitStack() as ph:
                        w1b = [sb(ph, f"w1b{i}", [128, 8, 256], BF16) for i in range(2)]
                        w3b = [sb(ph, f"w3b{i}", [128, 8, 256], BF16) for i in range(2)]
                        w2b = [sb(ph, f"w2b{i}", [128, 2, D], BF16) for i in range(2)]
                        act_ = [sb(ph, f"act{i}", [128, 2, T], BF16) for i in range(2)]
                        st_ = [sb(ph, f"st{i}", [128, 512]) for i in range(2)]
                        evt = [sb(ph, f"evt{i}", [128, 512]) for i in range(2)]
                        dj = 0
                        ej = 0
                        dvr = 0
                        pt_ = [sb(ph, f"pt{i}", [128, 512]) for i in range(2)] if moe else None
                        cbt = [sb(ph, f"cbt{i}", [128, T]) for i in range(2)] if moe else None
                        dg = [sb(ph, f"dg{i}", [128, 128]) for i in range(2)] if moe else None
                        sm = sb(ph, "sm", [128, 8]) if moe else None
                        if moe and run("ffn"):
                            for tt in (range(18) if not last else range(2, 18)):
                                L = lg[:, tt, :]
                                Cb = comb[:, tt, :]
                                S.op("dve", lambda e: e.reduce_max(sm[:, 0:1], L, AX.X), reads=["lg", "sm"], writes=["sm"])
                                S.op("dve", lambda e: e.tensor_scalar(Cb, L, sm[:, 0:1], None, ALU.is_equal), reads=["lg", "sm"], writes=["comb"])
                                S.op("dve", lambda e: e.scalar_tensor_tensor(L, Cb, -1e30, L, ALU.mult, ALU.add), reads=["comb", "lg"], writes=["lg"])
                                S.op("dve", lambda e: e.reduce_max(sm[:, 1:2], L, AX.X), reads=["lg", "sm"], writes=["sm"])
                                S.op("dve", lambda e: e.tensor_scalar(L, L, sm[:, 1:2], None, ALU.is_equal), reads=["lg", "sm"], writes=["lg"])
                                S.op("dve", lambda e: e.tensor_tensor(sm[:, 2:3], sm[:, 1:2], sm[:, 0:1], ALU.subtract), reads=["sm"], writes=["sm"])
                                S.op("act", lambda e: e.activation(out=sm[:, 3:4], in_=sm[:, 2:3], func=AF.Exp), reads=["sm"], writes=["sm"])
                                S.op("dve", lambda e: e.tensor_scalar(sm[:, 4:5], sm[:, 3:4], 1.0, None, ALU.add), reads=["sm"], writes=["sm"])
                                S.op("dve", lambda e: e.reciprocal(sm[:, 5:6], sm[:, 4:5]), reads=["sm"], writes=["sm"])
                                S.op("dve", lambda e: e.tensor_tensor(sm[:, 6:7], sm[:, 3:4], sm[:, 5:6], ALU.mult), reads=["sm"], writes=["sm"])
                                S.op("dve", lambda e: e.tensor_scalar(Cb, Cb, sm[:, 5:6], None, ALU.mult), reads=["comb", "sm"], writes=["comb"])
                                S.op("dve", lambda e: e.scalar_tensor_tensor(Cb, L, sm[:, 6:7], Cb, ALU.mult, ALU.add), reads=["comb", "lg", "sm"], writes=["comb"])
                        experts = range(NE) if moe else range(1)
                        gi = 0

                        def wsrc(e_):
                            if moe:
                                return moe_w1[jj, e_], moe_w3[jj, e_], moe_w2[jj, e_]
                            return ffn_w1[jj], ffn_w3[jj], ffn_w2[jj]

                        def load_fg(e_, g, sl):
                            a1, a3, a2 = wsrc(e_)
                            a1v = a1.rearrange("(c p) f -> p c f", p=128)
                            a3v = a3.rearrange("(c p) f -> p c f", p=128)
                            a2v = a2.rearrange("(c p) d -> p c d", p=128)
                            S.dma("pool", f"ffw{sl}", [(w1b[sl][:], a1v[:, :, g * 256:(g + 1) * 256]),
                                                       (w3b[sl][:], a3v[:, :, g * 256:(g + 1) * 256]),
                                                       (w2b[sl][:], a2v[:, 2 * g:2 * g + 2, :])],
                                  writes=[("ffw", sl)])
                        seq_fg = [(e_, g) for e_ in experts for g in range(NFG)] if run("ffn") else []
                        if seq_fg:
                            load_fg(seq_fg[0][0], seq_fg[0][1], 0)
                        si = 0
                        for idx, (e_, g) in enumerate(seq_fg):
                            sl = idx % 2
                            if idx + 1 < len(seq_fg):
                                load_fg(seq_fg[idx + 1][0], seq_fg[idx + 1][1], (idx + 1) % 2)
                            if moe and g == 0:
                                cbe = cbt[e_ % 2]
                                for t in qtiles:
                                    t0, tn = TILES[t]
                                    b_ = bank("d")
                                    for q in range(tn // 128):
                                        tt = t0 // 128 + q
                                        d_ = dg[tt % 2]
                                        S.op("dve", lambda e: e.tensor_scalar(d_[:], ident, comb[:, tt, e_:e_ + 1], None, ALU.mult),
                                             reads=["comb", "const"], writes=[("dg", tt % 2)])
                                        S.op("pe", lambda e: e.matmul(ps[b_][:, q * 128:(q + 1) * 128], ones_1, d_[:], start=True, stop=True),
                                             reads=[("dg", tt % 2), "const"], writes=[psk[b_]])
                                    S.op("act", lambda e: e.activation(out=cbe[:, t0:t0 + tn], in_=ps[b_][:, :tn], func=AF.Identity), reads=[psk[b_]], writes=[("cbt", e_ % 2)])
                            ab = act_[sl]
                            for t in qtiles:
                                t0, tn = TILES[t]
                                for f in range(2):
                                    b1, b3 = bank("a"), bank("b")
                                    for k in range(8):
                                        S.op("pe", lambda e: e.matmul(ps[b1][:, :tn], w1b[sl][:, k, f * 128:(f + 1) * 128], u[:, k, t0:t0 + tn],
                                                                      start=(k == 0), stop=(k == 7)),
                                             reads=[("ffw", sl), uk(t)], writes=[psk[b1]])
                                    for k in range(8):
                                        S.op("pe", lambda e: e.matmul(ps[b3][:, :tn], w3b[sl][:, k, f * 128:(f + 1) * 128], u[:, k, t0:t0 + tn],
                                                                      start=(k == 0), stop=(k == 7)),
                                             reads=[("ffw", sl), uk(t)], writes=[psk[b3]])
                                    stt, sk = st_[si % 2], ("st", si % 2)
                                    S.op("act", lambda e: e.activation(out=stt[:, :tn], in_=ps[b1][:, :tn], func=AF.Silu), reads=[psk[b1]], writes=[sk])
                                    if moe:
                                        ptt, pk = pt_[si % 2], ("pt", si % 2)
                                        S.op("dve", lambda e: e.tensor_tensor(ptt[:, :tn], stt[:, :tn], ps[b3][:, :tn], ALU.mult),
                                             reads=[sk, psk[b3]], writes=[pk])
                                        S.op("dve", lambda e: e.tensor_tensor(ab[:, f, t0:t0 + tn], ptt[:, :tn], cbt[e_ % 2][:, t0:t0 + tn], ALU.mult),
                                             reads=[pk, ("cbt", e_ % 2)], writes=[("act", sl, t, f)])
                                    else:
                                        S.op("dve", lambda e: e.tensor_tensor(ab[:, f, t0:t0 + tn], stt[:, :tn], ps[b3][:, :tn], ALU.mult),
                                             reads=[sk, psk[b3]], writes=[("act", sl, t, f)])
                                    si += 1
                            for m in range(8):
                                for t in qtiles:
                                    t0, tn = TILES[t]
                                    n = tile_n(t, s)
                                    via_act = (dj % 8) in (1, 4, 6)
                                    dj += 1
                                    if via_act:
                                        b_ = bank("d")
                                    else:
                                        b_ = (4, 5, 0, 1, 2, 3)[dvr % 6]
                                        dvr += 1
                                    for f in range(2):
                                        S.op("pe", lambda e: e.matmul(ps[b_][:, :tn], w2b[sl][:, f, m * 128:(m + 1) * 128], ab[:, f, t0:t0 + tn],
                                                                      start=(f == 0), stop=(f == 1)),
                                             reads=[("ffw", sl), ("act", sl, t, f)], writes=[psk[b_]])
                                    if via_act:
                                        ev_, evk = evt[ej % 2], ("evt", ej % 2)
                                        ej += 1
                                        S.op("act", lambda e: e.activation(out=ev_[:, :tn], in_=ps[b_][:, :tn], func=AF.Identity, scale=M(l, 40 + m, n)),
                                             reads=[psk[b_], "mod"], writes=[evk])
                                        S.op("pool", lambda e: e.tensor_tensor(h[:, m, t0:t0 + tn], h[:, m, t0:t0 + tn], ev_[:, :tn], ALU.add),
                                             reads=[evk, hk(m, t)], writes=[hk(m, t)])
                                    else:
                                        S.op("dve", lambda e: e.scalar_tensor_tensor(h[:, m, t0:t0 + tn], ps[b_][:, :tn], M(l, 40 + m, n),
                                                                                     h[:, m, t0:t0 + tn], ALU.mult, ALU.add),
                                             reads=[psk[b_], hk(m, t)], writes=[hk(m, t)])
                        S.barrier()

                with ExitStack() as ph:
                    nl = (l + 1 < NL)
                    if run("ln2"):
                        layer_norm(l, s, 2, qtiles, ((l + 1, 8, 0) if nl else None), ph)
                    S.barrier()

            yv = yT[s].rearrange("(c p) t -> p c t", p=128)
            S.dma("sp", "yst", [(yv[:, 0:4, :], h[:, 0:4, CTX:]), (yv[:, 4:8, :], h[:, 4:8, CTX:])],
                  reads=[hk(c, t) for c in range(8) for t in range(5)], writes=["y"])
            S.final_wait("sp", "yst")
            S.barrier()
    return nc


def _rope_tables():
    rows = SEQ // 64
    row = np.repeat(np.arange(rows, dtype=np.float32), 64)
    col = np.tile(np.arange(64, dtype=np.float32), rows)
    inv = (10000.0 ** (-np.arange(16, dtype=np.float32) / 16)).astype(np.float32)
    ang = np.concatenate([row[:, None] * inv, col[:, None] * inv], -1)
    jj = (np.arange(128) % 64) // 2
    return np.ascontiguousarray(np.cos(ang)[:, jj].T.astype(np.float32)), np.ascontiguousarray(np.sin(ang)[:, jj].T.astype(np.float32))


def _host_layout(inp):
    f = lambda a: np.ascontiguousarray(np.asarray(a, dtype=np.float32))
    sh = {}
    sh["w_mod"] = f(inp["w_mod"])
    sh["b_modT"] = f(np.asarray(inp["b_mod"]).reshape(DEPTH, 48, 128).transpose(2, 0, 1))
    sh["w_in"] = f(inp["w_in"])
    sh["w_out"] = f(inp["w_out"])
    lam = np.stack([np.asarray(inp[k]) for k in ("lam_q1", "lam_k1", "lam_q2", "lam_k2")], 1)
    sh["lamqk"] = f(np.broadcast_to(lam[None], (128, DEPTH, 4, 64)))
    sh["sublnT"] = f(np.asarray(inp["subln_g"]).T)
    cw = np.asarray(inp["conv_w"]).reshape(DEPTH, 4, 4, 128)
    cbias = np.asarray(inp["conv_b"]).reshape(DEPTH, 1, 4, 128)
    sh["convT"] = f(np.concatenate([cw, cbias], 1).transpose(3, 0, 2, 1))
    rv = np.stack([np.asarray(inp[k]).reshape(DEPTH, 2, 4, 128) for k in ("rg_ba", "rg_bx", "rg_lambda")], -1)
    sh["rgvT"] = f(rv.transpose(3, 0, 1, 2, 4))
    gb = np.zeros((DEPTH, 2, 4, 2, 128, 128), np.float32)
    for g, k in enumerate(("rg_wa", "rg_wx")):
        w = np.asarray(inp[k])
        for j in range(4):
            gb[:, :, j, g, 0:64, 0:64] = w[:, :, 2 * j]
            gb[:, :, j, g, 64:128, 64:128] = w[:, :, 2 * j + 1]
    sh["gate_bd"] = gb
    ln = np.stack([np.asarray(inp[k]).reshape(DEPTH, 8, 128) for k in ("ln1_g", "ln1_b", "ln2_g", "ln2_b")], 1)
    sh["lnT"] = f(ln.transpose(3, 0, 1, 2))
    for k in ("ffn_w1", "ffn_w3", "ffn_w2", "moe_w1", "moe_w3", "moe_w2"):
        sh[k] = f(inp[k])
    sh["routerT"] = f(np.asarray(inp["moe_router"]).reshape(2, 8, 128, NE).transpose(2, 0, 1, 3))
    c, s_ = _rope_tables()
    sh["cosd"], sh["sind"] = c, s_
    cfm = np.zeros((128, 4, 128), np.float32)
    cfm[:, 0, :] = np.eye(128, dtype=np.float32)
    cfm[:, 1, :] = 1.0 / 1024
    cfm[:, 2, :] = 1.0 / 128
    cfm[:, 3, :] = 1.0
    sh["constf"] = cfm
    cbm = np.zeros((128, 2, 128), np.float32)
    cbm[:, 0, :] = 1.0
    for j in range(64):
        cbm[2 * j + 1, 1, 2 * j] = -1.0
        cbm[2 * j, 1, 2 * j + 1] = 1.0
    sh["constb"] = cbm.astype(ml_dtypes.bfloat16)
    return sh


def kernel(**inputs):
    NL, NSEQ, NC = CFG["NL"], CFG["NSEQ"], CFG["NCORES"]
    shared = _host_layout(inputs)
    x = np.asarray(inputs["x"], dtype=np.float32)
    ctx = np.asarray(inputs["ctx"], dtype=np.float32)
    c = np.asarray(inputs["c"], dtype=np.float32)
    c_ctx = np.asarray(inputs["c_ctx"], dtype=np.float32)
    in_maps = []
    for core in range(NC):
        bs = [core * NSEQ + i for i in range(NSEQ)]
        m = dict(shared)
        m["xT"] = np.ascontiguousarray(x[bs].transpose(0, 2, 1))
        m["ctxT"] = np.ascontiguousarray(ctx[bs].transpose(0, 2, 1))
        cols = [c[b] for b in bs]
        while len(cols) < 2:
            cols.append(cols[-1])
        cols.append(c_ctx)
        m["cT"] = np.ascontiguousarray(np.stack(cols, -1).reshape(8, 128, 3).transpose(1, 0, 2))
        in_maps.append(m)
    nc = build(NL, NSEQ)
    res = run_bass_kernel_spmd(nc, in_maps, core_ids=list(range(NC)))
    out = np.empty((NC * NSEQ, SEQ, D), np.float32)
    for core in range(NC):
        y = res.results[core]["yT"]
        for i in range(NSEQ):
            out[core * NSEQ + i] = y[i].T
    return out
```

```python
import math
from contextlib import ExitStack

import numpy as np
import ml_dtypes

import concourse.bass as bass
import concourse.mybir as mybir
from concourse.bass_utils import run_bass_kernel_spmd

F32 = mybir.dt.float32
BF16 = mybir.dt.bfloat16
ALU = mybir.AluOpType
AF = mybir.ActivationFunctionType
AX = mybir.AxisListType

D = 1024
DEPTH = 4
SEQ = 2048
CTX = 256
T = SEQ + CTX
IN_W = 2560
D_FF = 2816
NE = 8
ALPHA = (2 * DEPTH) ** 0.25
LN_EPS = 1e-5 / (ALPHA * ALPHA)
RMS_EPS = 1e-5
ATTN_SCALE = 64 ** -0.5
TILES = [(0, 256), (256, 512), (768, 512), (1280, 512), (1792, 512)]
NFG = D_FF // 256

CFG = {"NL": 4, "NSEQ": 2, "NCORES": 8, "STOP": "ln2"}
_ORDER = ["load", "rg", "attn", "ln1", "ffn", "ln2"]


def run(stage):
    return _ORDER.index(stage) <= _ORDER.index(CFG["STOP"])


class Sync:
    def __init__(self, nc, es):
        self.nc = nc
        self.eng = {"pe": nc.tensor, "act": nc.scalar, "dve": nc.vector, "pool": nc.gpsimd, "sp": nc.sync}
        self.sem, self.cnt, self.seen, self.state = {}, {}, {}, {}
        self.es = es
        for e in self.eng:
            self.newsem(e)

    def newsem(self, name):
        self.sem[name] = self.es.enter_context(self.nc.semaphore(name))
        self.cnt[name] = 0

    def _deps(self, me, reads, writes):
        deps = {}

        def add(p):
            if p is not None and deps.get(p[0], 0) < p[1]:
                deps[p[0]] = p[1]
        for k in reads:
            st = self.state.get(k)
            if st:
                add(st[0])
                if isinstance(k, tuple) and k[0] == "ps":
                    for s, v in st[1].items():
                        if s != me:
                            add((s, v))
        for k in writes:
            st = self.state.get(k)
            if st:
                add(st[0])
                for s, v in st[1].items():
                    add((s, v))
        return deps

    def _wait(self, me, deps):
        seen = self.seen.setdefault(me, {})
        for s, v in deps.items():
            if s == me and me == "pe":
                continue
            if seen.get(s, 0) >= v:
                continue
            self.eng[me].wait_ge(self.sem[s], v)
            seen[s] = v

    def _record(self, tag, reads, writes):
        for k in reads:
            st = self.state.setdefault(k, [None, {}])
            st[1][tag[0]] = tag[1]
        for k in writes:
            self.state[k] = [tag, {}]

    def op(self, me, fn, reads=(), writes=()):
        self._wait(me, self._deps(me, reads, writes))
        fn(self.eng[me]).then_inc(self.sem[me], 1)
        self.cnt[me] += 1
        self._record((me, self.cnt[me]), reads, writes)

    def dma(self, queue, semname, xfers, reads=(), writes=()):
        if semname not in self.sem:
            self.newsem(semname)
        self._wait(queue, self._deps(semname, reads, writes))
        for (o, i) in xfers:
            self.eng[queue].dma_start(out=o, in_=i).then_inc(self.sem[semname], 16)
            self.cnt[semname] += 16
        self._record((semname, self.cnt[semname]), reads, writes)

    def barrier(self):
        for me in self.eng:
            self._wait(me, {s: v for s, v in self.cnt.items() if v > 0 and s != me})

    def final_wait(self, me, semname):
        self._wait(me, {semname: self.cnt[semname]})


def build(NL, NSEQ):
    nc = bass.Bass("TRN2", target_bir_lowering=False)

    def din(name, shape, dt=F32):
        return nc.dram_tensor(name, list(shape), dt, kind="ExternalInput").ap()

    xT = din("xT", [NSEQ, D, SEQ])
    ctxT = din("ctxT", [NSEQ, D, CTX])
    cT = din("cT", [128, 8, 3])
    w_mod = din("w_mod", [DEPTH, D, 6 * D])
    b_modT = din("b_modT", [128, DEPTH, 48])
    w_in = din("w_in", [DEPTH, D, IN_W])
    w_out = din("w_out", [DEPTH, D, D])
    lamqk = din("lamqk", [128, DEPTH, 4, 64])
    sublnT = din("sublnT", [128, DEPTH])
    convT = din("convT", [128, DEPTH, 4, 5])
    rgvT = din("rgvT", [128, DEPTH, 2, 4, 3])
    gate_bd = din("gate_bd", [DEPTH, 2, 4, 2, 128, 128])
    lnT = din("lnT", [128, DEPTH, 4, 8])
    ffn_w1 = din("ffn_w1", [2, D, D_FF])
    ffn_w3 = din("ffn_w3", [2, D, D_FF])
    ffn_w2 = din("ffn_w2", [2, D_FF, D])
    routerT = din("routerT", [128, 2, 8, NE])
    moe_w1 = din("moe_w1", [2, NE, D, D_FF])
    moe_w3 = din("moe_w3", [2, NE, D, D_FF])
    moe_w2 = din("moe_w2", [2, NE, D_FF, D])
    cosd = din("cosd", [128, SEQ])
    sind = din("sind", [128, SEQ])
    constf = din("constf", [128, 4, 128])
    constb = din("constb", [128, 2, 128], BF16)
    yT = nc.dram_tensor("yT", [NSEQ, D, SEQ], F32, kind="ExternalOutput").ap()

    es = ExitStack()
    with es:
        S = Sync(nc, es)

        uid = [0]

        def sb(scope, name, shape, dt=F32):
            uid[0] += 1
            return scope.enter_context(nc.sbuf_tensor(f"{name}_{uid[0]}", list(shape), dt))

        ps = [es.enter_context(nc.psum_tensor(f"ps{i}", [128, 512], F32)) for i in range(8)]
        psk = [("ps", i) for i in range(8)]
        rr = {"a": 0, "b": 0, "c": 0, "d": 0}

        def bank(role):
            base = {"a": 0, "b": 2, "c": 4, "d": 6}[role]
            i = base + rr[role] % 2
            rr[role] += 1
            return i

        h = sb(es, "h", [128, 8, T])
        u = sb(es, "u", [128, 8, T], BF16)
        cos = sb(es, "cos", [128, SEQ])
        sin = sb(es, "sin", [128, SEQ])
        cf = sb(es, "cf", [128, 4, 128])
        cb16 = sb(es, "cb16", [128, 2, 128], BF16)
        mod = sb(es, "mod", [128, DEPTH, 48, 3])
        lnp = sb(es, "lnp", [128, DEPTH, 4, 8])
        convp = sb(es, "convp", [128, DEPTH, 4, 5])
        rgv = sb(es, "rgv", [128, DEPTH, 2, 4, 3])
        rgc = sb(es, "rgc", [128, DEPTH, 2, 4])
        subg = sb(es, "subg", [128, DEPTH])
        lamv = sb(es, "lamv", [128, DEPTH, 4])
        rout = sb(es, "rout", [128, 2, 8, NE])
        ident = cf[:, 0, :]
        ones_d = cf[:, 1, :]
        ones_h = cf[:, 2, :]
        ones_1 = cf[:, 3, :]
        onesb = cb16[:, 0, :]
        Rb = cb16[:, 1, :]

        S.dma("sp", "const", [(cos[:], cosd[:, :]), (sin[:], sind[:, :]), (cf[:], constf[:, :, :]),
                              (cb16[:], constb[:, :, :]), (lnp[:], lnT[:, :, :, :]), (convp[:], convT[:, :, :, :]),
                              (rgv[:], rgvT[:, :, :, :, :]), (subg[:], sublnT[:, :]),
                              (rout[:], routerT[:, :, :, :])],
              writes=["const"])

        def M(l, q, n):
            return mod[:, l, q, n:n + 1]

        with ExitStack() as ph:
            scT = sb(ph, "scT", [128, 8, 3])
            bmod = sb(ph, "bmod", [128, DEPTH, 48])
            wm = [sb(ph, f"wm{i}", [128, 8, 512]) for i in range(2)]
            lamt = sb(ph, "lamt", [128, DEPTH, 4, 64])
            S.dma("sp", "cld", [(scT[:], cT[:, :, :]), (bmod[:], b_modT[:, :, :]), (lamt[:], lamqk[:, :, :, :])], writes=["scT", "bmod", "lamt"])
            S.op("act", lambda e: e.activation(out=scT[:], in_=scT[:], func=AF.Silu), reads=["scT"], writes=["scT"])
            it = 0
            for l in range(NL):
                wv_ = w_mod[l].rearrange("(c p) f -> p c f", p=128)
                for j in range(12):
                    sl = it % 2
                    it += 1
                    S.dma("sp", f"wm{sl}", [(wm[sl][:, 0:4, :], wv_[:, 0:4, j * 512:(j + 1) * 512]),
                                            (wm[sl][:, 4:8, :], wv_[:, 4:8, j * 512:(j + 1) * 512])],
                          writes=[("wm", sl)])
                    for fc in range(4):
                        b_ = bank("d")
                        for k in range(8):
                            S.op("pe", lambda e: e.matmul(ps[b_][:, 0:3], wm[sl][:, k, fc * 128:(fc + 1) * 128], scT[:, k, :],
                                                          start=(k == 0), stop=(k == 7)),
                                 reads=[("wm", sl), "scT"], writes=[psk[b_]])
                        q = j * 4 + fc
                        S.op("dve", lambda e: e.tensor_scalar(mod[:, l, q, :], ps[b_][:, 0:3], bmod[:, l, q:q + 1], None, ALU.add),
                             reads=[psk[b_], "bmod"], writes=["mod"])
                for g0 in (8, 32):
                    S.op("dve", lambda e: e.tensor_scalar(mod[:, l, g0:g0 + 8, :], mod[:, l, g0:g0 + 8, :], 1.0, None, ALU.add),
                         reads=["mod"], writes=["mod"])
                for g0 in (16, 40):
                    S.op("dve", lambda e: e.tensor_scalar(mod[:, l, g0:g0 + 8, :], mod[:, l, g0:g0 + 8, :], 1.0, 1.0 / ALPHA,
                                                          ALU.add, ALU.mult),
                         reads=["mod"], writes=["mod"])
            S.op("act", lambda e: e.activation(out=rgc[:], in_=rgv[:, :, :, :, 2], func=AF.Exp, scale=-1.0),
                 reads=["const"], writes=["rgc"])
            S.op("dve", lambda e: e.tensor_scalar(rgc[:], rgc[:], 1.0, None, ALU.add), reads=["rgc"], writes=["rgc"])
            S.op("act", lambda e: e.activation(out=rgc[:], in_=rgc[:], func=AF.Ln), reads=["rgc"], writes=["rgc"])
            S.op("dve", lambda e: e.tensor_scalar(rgc[:], rgc[:], -8.0, None, ALU.mult), reads=["rgc"], writes=["rgc"])
            for l in range(NL):
                lam_init = 0.8 - 0.6 * math.exp(-0.3 * l)
                S.op("dve", lambda e: e.tensor_tensor(lamt[:, l, 0, :], lamt[:, l, 0, :], lamt[:, l, 1, :], ALU.mult),
                     reads=["lamt"], writes=["lamt"])
                S.op("dve", lambda e: e.tensor_tensor(lamt[:, l, 2, :], lamt[:, l, 2, :], lamt[:, l, 3, :], ALU.mult),
                     reads=["lamt"], writes=["lamt"])
                S.op("dve", lambda e: e.reduce_sum(lamv[:, l, 0:1], lamt[:, l, 0, :], AX.X), reads=["lamt"], writes=["lamv"])
                S.op("dve", lambda e: e.reduce_sum(lamv[:, l, 1:2], lamt[:, l, 2, :], AX.X), reads=["lamv", "lamt"], writes=["lamv"])
                S.op("act", lambda e: e.activation(out=lamv[:, l, 0:2], in_=lamv[:, l, 0:2], func=AF.Exp),
                     reads=["lamv"], writes=["lamv"])
                S.op("dve", lambda e: e.scalar_tensor_tensor(lamv[:, l, 2:3], lamv[:, l, 1:2], -lam_init, lamv[:, l, 0:1],
                                                             ALU.add, ALU.subtract),
                     reads=["lamv"], writes=["lamv"])
                S.op("dve", lambda e: e.tensor_scalar(subg[:, l:l + 1], subg[:, l:l + 1], 1.0 - lam_init, None, ALU.mult),
                     reads=["const", "lamv"], writes=["subg"])
            S.barrier()

        def tile_n(t, s):
            return 2 if t == 0 else s

        def hk(c, t):
            return ("h", c, t)

        def uk(t):
            return ("u", t)

        def out_proj_partial(l, s, t, wo, wokey, mixt, mixkey, role="c", evs=None):
            t0, tn = TILES[t]
            n = tile_n(t, s)
            for m in range(8):
                if evs is not None and m % 3 == 1:
                    b_ = bank("d")
                    S.op("pe", lambda e: e.matmul(ps[b_][:, :tn], wo[:, m * 128:(m + 1) * 128], mixt[:, :tn], start=True, stop=True),
                         reads=[wokey, mixkey], writes=[psk[b_]])
                    ev_, evk = evs[m % 2], ("oevt", m % 2)
                    S.op("act", lambda e: e.activation(out=ev_[:, :tn], in_=ps[b_][:, :tn], func=AF.Identity, scale=M(l, 16 + m, n)),
                         reads=[psk[b_], "mod"], writes=[evk])
                    S.op("pool", lambda e: e.tensor_tensor(h[:, m, t0:t0 + tn], h[:, m, t0:t0 + tn], ev_[:, :tn], ALU.add),
                         reads=[evk, hk(m, t)], writes=[hk(m, t)])
                    continue
                b_ = bank(role)
                S.op("pe", lambda e: e.matmul(ps[b_][:, :tn], wo[:, m * 128:(m + 1) * 128], mixt[:, :tn], start=True, stop=True),
                     reads=[wokey, mixkey], writes=[psk[b_]])
                S.op("dve", lambda e: e.scalar_tensor_tensor(h[:, m, t0:t0 + tn], ps[b_][:, :tn], M(l, 16 + m, n),
                                                             h[:, m, t0:t0 + tn], ALU.mult, ALU.add),
                     reads=[psk[b_], hk(m, t)], writes=[hk(m, t)])

        def layer_norm(l, s, which, tiles, nxt, ph, router_j=None, lg=None):
            gq, bq = (0, 1) if which == 1 else (2, 3)
            tmp = [sb(ph, f"ln_tmp{i}", [128, 512]) for i in range(2)]
            msb = sb(ph, "ln_m", [128, 512])
            rstd = sb(ph, "ln_r", [128, 512])
            sq = [sb(ph, f"ln_sq{i}", [128, 512]) for i in range(2)]
            u32 = [sb(ph, f"ln_u32{i}", [128, 512]) for i in range(2)] if router_j is not None else None
            lgT = sb(ph, "ln_lgT", [8, 512]) if router_j is not None else None
            for t in tiles:
                t0, tn = TILES[t]
                n = tile_n(t, s)
                bm, bv = bank("d"), bank("d")
                for c in range(8):
                    S.op("pe", lambda e: e.matmul(ps[bm][:, :tn], ones_d, h[:, c, t0:t0 + tn], start=(c == 0), stop=(c == 7)),
                         reads=[hk(c, t), "const"], writes=[psk[bm]])
                for c in range(8):
                    sq_ = sq[c % 2]
                    S.op("pool", lambda e: e.tensor_tensor(sq_[:, :tn], h[:, c, t0:t0 + tn], h[:, c, t0:t0 + tn], ALU.mult),
                         reads=[hk(c, t)], writes=[("lnsq", c % 2)])
                    S.op("pe", lambda e: e.matmul(ps[bv][:, :tn], ones_d, sq_[:, :tn], start=(c == 0), stop=(c == 7)),
                         reads=[("lnsq", c % 2), "const"], writes=[psk[bv]])
                S.op("act", lambda e: e.activation(out=msb[:, :tn], in_=ps[bm][:, :tn], func=AF.Identity), reads=[psk[bm]], writes=["lnm"])
                S.op("dve", lambda e: e.tensor_tensor(rstd[:, :tn], msb[:, :tn], msb[:, :tn], ALU.mult), reads=["lnm"], writes=["lnr"])
                S.op("dve", lambda e: e.scalar_tensor_tensor(rstd[:, :tn], ps[bv][:, :tn], LN_EPS, rstd[:, :tn], ALU.add, ALU.subtract),
                     reads=[psk[bv], "lnr"], writes=["lnr"])
                S.op("act", lambda e: e.activation(out=rstd[:, :tn], in_=rstd[:, :tn], func=AF.Ln), reads=["lnr"], writes=["lnr"])
                S.op("act", lambda e: e.activation(out=rstd[:, :tn], in_=rstd[:, :tn], func=AF.Exp, scale=-0.5), reads=["lnr"], writes=["lnr"])
                if router_j is not None:
                    bl = bank("c")
                for c in range(8):
                    tm = tmp[c % 2]
                    hv = h[:, c, t0:t0 + tn]
                    S.op("dve", lambda e: e.tensor_tensor(tm[:, :tn], hv, msb[:, :tn], ALU.subtract),
                         reads=[hk(c, t), "lnm"], writes=[("lntmp", c % 2)])
                    S.op("dve", lambda e: e.tensor_tensor(tm[:, :tn], tm[:, :tn], rstd[:, :tn], ALU.mult),
                         reads=[("lntmp", c % 2), "lnr"], writes=[("lntmp", c % 2)])
                    S.op("act", lambda e: e.activation(out=hv, in_=tm[:, :tn], func=AF.Identity,
                                                       bias=lnp[:, l, bq, c:c + 1], scale=lnp[:, l, gq, c:c + 1]),
                         reads=[("lntmp", c % 2), "const"], writes=[hk(c, t)])
                    if nxt is not None:
                        l2, oq, sq0 = nxt
                        S.op("act", lambda e: e.activation(out=u[:, c, t0:t0 + tn], in_=hv, func=AF.Identity,
                                                           bias=M(l2, sq0 + c, n), scale=M(l2, oq + c, n)),
                             reads=[hk(c, t), "mod"], writes=[uk(t)])
                        if router_j is not None:
                            uu = u32[c % 2]
                            S.op("dve", lambda e: e.tensor_scalar(uu[:, :tn], hv, M(l2, oq + c, n), M(l2, sq0 + c, n), ALU.mult, ALU.add),
                                 reads=[hk(c, t), "mod"], writes=[("u32", c % 2)])
                            S.op("pe", lambda e: e.matmul(ps[bl][0:8, :tn], rout[:, router_j, c, :], uu[:, :tn], start=(c == 0), stop=(c == 7)),
                                 reads=[("u32", c % 2), "const"], writes=[psk[bl]])
                if router_j is not None:
                    S.op("act", lambda e: e.activation(out=lgT[:, :tn], in_=ps[bl][0:8, :tn], func=AF.Identity), reads=[psk[bl]], writes=["lgT"])
                    for q in range(tn // 128):
                        b2 = bank("d")
                        S.op("pe", lambda e: e.matmul(ps[b2][:, 0:8], lgT[:, q * 128:(q + 1) * 128], ident[0:8, 0:8], start=True, stop=True),
                             reads=["lgT", "const"], writes=[psk[b2]])
                        tt = t0 // 128 + q
                        S.op("dve", lambda e: e.tensor_copy(lg[:, tt, :], ps[b2][:, 0:8]), reads=[psk[b2]], writes=["lg"])

        for s in range(NSEQ):
            xv = xT[s].rearrange("(c p) t -> p c t", p=128)
            cv = ctxT[s].rearrange("(c p) t -> p c t", p=128)
            allh = [hk(c, t) for c in range(8) for t in range(5)]
            S.dma("sp", "hld", [(h[:, 0:4, CTX:], xv[:, 0:4, :]), (h[:, 4:8, CTX:], xv[:, 4:8, :]), (h[:, :, 0:CTX], cv[:, :, :])],
                  writes=allh)
            for t in range(5):
                t0, tn = TILES[t]
                n = tile_n(t, s)
                for c in range(8):
                    S.op("dve", lambda e: e.tensor_scalar(u[:, c, t0:t0 + tn], h[:, c, t0:t0 + tn], M(0, 8 + c, n), M(0, c, n), ALU.mult, ALU.add),
                         reads=[hk(c, t), "mod"], writes=[uk(t)])

            for l in range(NL):
                last = (l == DEPTH - 1)
                qtiles = [1, 2, 3, 4] if last else [0, 1, 2, 3, 4]
                win = w_in[l].rearrange("(c p) f -> p c f", p=128)
                wout = w_out[l]

                with ExitStack() as ph:
                    xrp = sb(ph, "xrp", [128, T + 8])
                    xc = sb(ph, "xc", [128, T])
                    A = sb(ph, "A", [128, T])
                    Bm = sb(ph, "Bm", [128, T])
                    H = sb(ph, "H", [128, T])
                    wxr = [sb(ph, f"wxr{i}", [128, 8, 128], BF16) for i in range(2)]
                    wy = [sb(ph, f"wy{i}", [128, 8, 128], BF16) for i in range(2)]
                    wo = [sb(ph, f"wo{i}", [128, D], BF16) for i in range(2)]
                    wg = [sb(ph, f"wg{i}", [128, 4, 128]) for i in range(2)]
                    tmpf = [sb(ph, f"rtmp{i}", [128, 512]) for i in range(2)]
                    oev = [sb(ph, f"oev{i}", [128, 512]) for i in range(2)]
                    mixt = [sb(ph, f"rmix{i}", [128, 512], BF16) for i in range(2)]
                    mi = 0
                    CO, LO = 2, 261

                    def load_rg(j):
                        sl = j % 2
                        S.dma("pool", f"rgwp{sl}", [(wxr[sl][:], win[:, :, 1024 + j * 128:1024 + (j + 1) * 128]),
                                                    (wy[sl][:], win[:, :, 2048 + j * 128:2048 + (j + 1) * 128]),
                                                    (wo[sl][:], wout[(4 + j) * 128:(5 + j) * 128, :])],
                              writes=[("rgwp", sl)])
                        S.dma("sp", f"rgws{sl}", [(wg[sl][:, d * 2 + g, :], gate_bd[l, d, j, g]) for d in range(2) for g in range(2)],
                              writes=[("rgws", sl)])
                    if run("rg"):
                        load_rg(0)
                    for j in range(4 if run("rg") else 0):
                        sl = j % 2
                        if j + 1 < 4:
                            load_rg(j + 1)
                        S.op("pool", lambda e: e.memset(xrp[:, 0:2], 0.0), writes=["xrp"])
                        S.op("pool", lambda e: e.memset(xrp[:, 258:261], 0.0), reads=["xrp"], writes=["xrp"])
                        S.op("pool", lambda e: e.memset(xrp[:, 2309:2312], 0.0), reads=["xrp"], writes=["xrp"])
                        for t in range(5):
                            t0, tn = TILES[t]
                            b_ = bank("a")
                            for k in range(8):
                                S.op("pe", lambda e: e.matmul(ps[b_][:, :tn], wxr[sl][:, k, :], u[:, k, t0:t0 + tn], start=(k == 0), stop=(k == 7)),
                                     reads=[("rgwp", sl), uk(t)], writes=[psk[b_]])
                            o0 = CO if t == 0 else LO + (t0 - CTX)
                            S.op("act", lambda e: e.activation(out=xrp[:, o0:o0 + tn], in_=ps[b_][:, :tn], func=AF.Identity), reads=[psk[b_], "xrp"], writes=["xrp"])
                        for (dst0, n_, src0) in ((0, CTX, CO - 2), (CTX, SEQ, LO - 2)):
                            S.op("dve", lambda e: e.tensor_scalar(xc[:, dst0:dst0 + n_], xrp[:, src0:src0 + n_], convp[:, l, j, 0:1],
                                                                  convp[:, l, j, 4:5], ALU.mult, ALU.add),
                                 reads=["xrp", "const"], writes=["xc"])
                            for tap in range(1, 4):
                                S.op("dve", lambda e: e.scalar_tensor_tensor(xc[:, dst0:dst0 + n_], xrp[:, src0 + tap:src0 + tap + n_],
                                                                             convp[:, l, j, tap:tap + 1], xc[:, dst0:dst0 + n_], ALU.mult, ALU.add),
                                     reads=["xrp", "xc"], writes=["xc"])
                        for d in range(2):
                            for t in range(5):
                                t0, tn = TILES[t]
                                for g, dst, key in ((0, A, "A"), (1, Bm, "B")):
                                    b_ = bank("b")
                                    S.op("pe", lambda e: e.matmul(ps[b_][:, :tn], wg[sl][:, d * 2 + g, :], xc[:, t0:t0 + tn], start=True, stop=True),
                                         reads=[("rgws", sl), "xc"], writes=[psk[b_]])
                                    S.op("act", lambda e: e.activation(out=dst[:, t0:t0 + tn], in_=ps[b_][:, :tn], func=AF.Sigmoid,
                                                                       bias=rgv[:, l, d, j, g:g + 1]),
                                         reads=[psk[b_], "const"], writes=[key])
                            S.op("act", lambda e: e.activation(out=A[:], in_=A[:], func=AF.Exp, scale=rgc[:, l, d, j:j + 1]),
                                 reads=["A", "rgc"], writes=["A"])
                            S.op("dve", lambda e: e.tensor_tensor(Bm[:], Bm[:], xc[:], ALU.mult), reads=["B", "xc"], writes=["B"])
                            scr = xrp[:, 0:T]
                            S.op("act", lambda e: e.activation(out=scr, in_=A[:], func=AF.Square), reads=["A", "xrp"], writes=["xrp"])
                            S.op("dve", lambda e: e.tensor_scalar(scr, scr, -1.0, 1.0, ALU.mult, ALU.add), reads=["xrp"], writes=["xrp"])
                            S.op("dve", lambda e: e.tensor_scalar(scr, scr, 0.0, None, ALU.max), reads=["xrp"], writes=["xrp"])
                            S.op("act", lambda e: e.activation(out=scr, in_=scr, func=AF.Sqrt), reads=["xrp"], writes=["xrp"])
                            S.op("dve", lambda e: e.tensor_tensor(Bm[:], Bm[:], scr, ALU.mult), reads=["B", "xrp"], writes=["B"])
                            if d == 0:
                                S.op("dve", lambda e: e.tensor_tensor_scan(H[:, 0:CTX], A[:, 0:CTX], Bm[:, 0:CTX], 0.0, ALU.mult, ALU.add),
                                     reads=["A", "B"], writes=["H"])
                                S.op("dve", lambda e: e.tensor_tensor_scan(H[:, CTX:], A[:, CTX:], Bm[:, CTX:], H[:, CTX - 1:CTX], ALU.mult, ALU.add),
                                     reads=["A", "B", "H"], writes=["H"])
                            else:
                                C = xrp
                                S.op("dve", lambda e: e.tensor_tensor_scan(C[:, 0:CTX][:, ::-1], A[:, 0:CTX][:, ::-1], Bm[:, 0:CTX][:, ::-1],
                                                                           0.0, ALU.mult, ALU.add),
                                     reads=["A", "B", "xrp"], writes=["xrp"])
                                S.op("dve", lambda e: e.tensor_tensor_scan(C[:, CTX:T][:, ::-1], A[:, CTX:][:, ::-1], Bm[:, CTX:][:, ::-1],
                                                                           C[:, 0:1], ALU.mult, ALU.add),
                                     reads=["A", "B", "xrp"], writes=["xrp"])
                                S.op("dve", lambda e: e.tensor_tensor(H[:], H[:], C[:, 0:T], ALU.add), reads=["H", "xrp"], writes=["H"])
                        for t in qtiles:
                            t0, tn = TILES[t]
                            b_ = bank("a")
                            for k in range(8):
                                S.op("pe", lambda e: e.matmul(ps[b_][:, :tn], wy[sl][:, k, :], u[:, k, t0:t0 + tn], start=(k == 0), stop=(k == 7)),
                                     reads=[("rgwp", sl), uk(t)], writes=[psk[b_]])
                            tf, mx = tmpf[mi % 2], mixt[mi % 2]
                            kf, km = ("rtmp", mi % 2), ("rmix", mi % 2)
                            mi += 1
                            S.op("act", lambda e: e.activation(out=tf[:, :tn], in_=ps[b_][:, :tn], func=AF.Gelu_apprx_tanh),
                                 reads=[psk[b_]], writes=[kf])
                            S.op("dve", lambda e: e.tensor_tensor(mx[:, :tn], tf[:, :tn], H[:, t0:t0 + tn], ALU.mult), reads=[kf, "H"], writes=[km])
                            out_proj_partial(l, s, t, wo[sl], ("rgwp", sl), mx, km, evs=oev)
                    S.barrier()

                with ExitStack() as ph:
                    v = sb(ph, "v", [128, 18, 512], BF16)
                    wv = sb(ph, "wv", [128, 8, 512], BF16)
                    kh = sb(ph, "kh", [128, T], BF16)
                    qz = [sb(ph, f"qz{i}", [128, T], BF16) for i in range(2)]
                    wk = [sb(ph, f"wk{i}", [128, 8, 128], BF16) for i in range(2)]
                    wq = [sb(ph, f"wq{i}", [128, 8, 128], BF16) for i in range(2)]
                    wo = [sb(ph, f"awo{i}", [128, D], BF16) for i in range(2)]
                    E = [sb(ph, f"E{i}", [128, 512], BF16) for i in range(3)]
                    qb = [sb(ph, f"qb{i}", [128, 512], BF16) for i in range(2)]
                    rm = [sb(ph, f"rm{i}", [128, 512]) for i in range(2)]
                    at = sb(ph, "at", [128, 512])
                    sqt = sb(ph, "sqt", [128, 512])
                    rs = sb(ph, "rs", [128, 512])
                    r1t, r2t = [at], [sqt]
                    mixt = [sb(ph, f"amix{i}", [128, 512], BF16) for i in range(2)]
                    if run("attn"):
                        S.op("pool", lambda e: e.memset(qz[0][64:128, :], 0.0), writes=["qh"])
                        S.op("pool", lambda e: e.memset(qz[1][0:64, :], 0.0), writes=["qh"])

                    def load_head(hd):
                        sl = hd % 2
                        S.dma("pool", f"attw{sl}", [(wk[sl][:], win[:, :, hd * 128:(hd + 1) * 128]),
                                                    (wq[sl][:], win[:, :, 1536 + hd * 128:1536 + (hd + 1) * 128]),
                                                    (wo[sl][:], wout[hd * 128:(hd + 1) * 128, :])],
                              writes=[("attw", sl)])
                    if run("attn"):
                        S.dma("pool", "wv", [(wv[:], win[:, :, 512:1024])], writes=["wv"])
                        load_head(0)
                    for tt in range(18 if (run("attn") and CFG.get("ASUB", 9) >= 1) else 0):
                        b_ = bank("a")
                        tile_of = 0 if tt < 2 else 1 + (tt - 2) // 4
                        for k in range(8):
                            S.op("pe", lambda e: e.matmul(ps[b_][:, :], u[:, k, tt * 128:(tt + 1) * 128], wv[:, k, :], start=(k == 0), stop=(k == 7)),
                                 reads=["wv", uk(tile_of)], writes=[psk[b_]])
                        S.op("act", lambda e: e.activation(out=v[:, tt, :], in_=ps[b_][:, :], func=AF.Identity), reads=[psk[b_]], writes=["v"])
                    ei = 0
                    qi = 0
                    mi = 0
                    ASUB = CFG.get("ASUB", 9)
                    for hd in range(4 if (run("attn") and ASUB >= 2) else 0):
                        sl = hd % 2
                        if hd + 1 < 4:
                            load_head(hd + 1)

                        def proj_rope(wt, dst, dkey, tiles):
                            nonlocal qi
                            for t in tiles:
                                t0, tn = TILES[t]
                                b_ = bank("a")
                                for k in range(8):
                                    S.op("pe", lambda e: e.matmul(ps[b_][:, :tn], wt[:, k, :], u[:, k, t0:t0 + tn], start=(k == 0), stop=(k == 7)),
                                         reads=[("attw", sl), uk(t)], writes=[psk[b_]])
                                if t == 0:
                                    if isinstance(dst, list):
                                        S.op("act", lambda e: e.activation(out=dst[0][0:64, t0:t0 + tn], in_=ps[b_][0:64, :tn], func=AF.Identity), reads=[psk[b_]], writes=[dkey])
                                        S.op("act", lambda e: e.activation(out=dst[1][64:128, t0:t0 + tn], in_=ps[b_][64:128, :tn], func=AF.Identity), reads=[psk[b_]], writes=[dkey])
                                    else:
                                        S.op("act", lambda e: e.activation(out=dst[:, t0:t0 + tn], in_=ps[b_][:, :tn], func=AF.Identity), reads=[psk[b_]], writes=[dkey])
                                    continue
                                i2 = qi % 2
                                qi += 1
                                l0 = t0 - CTX
                                S.op("act", lambda e: e.activation(out=qb[i2][:, :tn], in_=ps[b_][:, :tn], func=AF.Identity), reads=[psk[b_]], writes=[("qb", i2)])
                                b2 = bank("b")
                                S.op("pe", lambda e: e.matmul(ps[b2][:, :tn], Rb, qb[i2][:, :tn], start=True, stop=True),
                                     reads=[("qb", i2), "const"], writes=[psk[b2]])
                                RSUB = CFG.get("RSUB", 9)
                                if RSUB < 2:
                                    continue
                                S.op("dve", lambda e: e.tensor_tensor(r1t[0][:, :tn], cos[:, l0:l0 + tn], ps[b_][:, :tn], ALU.mult),
                                     reads=[psk[b_], "const", ("qb", i2)], writes=["at"])
                                if RSUB == 2:
                                    continue
                                S.op("dve", lambda e: e.tensor_tensor(r2t[0][:, :tn], sin[:, l0:l0 + tn], ps[b2][:, :tn], ALU.mult),
                                     reads=[psk[b2], "const"], writes=["sqt"])
                                if RSUB < 3:
                                    continue
                                if isinstance(dst, list):
                                    S.op("dve", lambda e: e.tensor_tensor(dst[0][0:64, t0:t0 + tn], r1t[0][0:64, :tn], r2t[0][0:64, :tn], ALU.add),
                                         reads=["at", "sqt"], writes=[dkey])
                                    S.op("dve", lambda e: e.tensor_tensor(dst[1][64:128, t0:t0 + tn], r1t[0][64:128, :tn], r2t[0][64:128, :tn], ALU.add),
                                         reads=["at", "sqt"], writes=[dkey])
                                else:
                                    S.op("dve", lambda e: e.tensor_tensor(dst[:, t0:t0 + tn], r1t[0][:, :tn], r2t[0][:, :tn], ALU.add),
                                         reads=["at", "sqt"], writes=[dkey])
                        proj_rope(wk[sl], kh, "kh", [0, 1, 2, 3, 4] if ASUB >= 3 else [0])
                        if ASUB >= 4:
                            proj_rope(wq[sl], qz, "qh", qtiles)
                        LA = 2
                        steps = []
                        for t in (qtiles if ASUB >= 5 else []):
                            kts = [0, 1] if t == 0 else list(range(18))
                            for mp in range(2):
                                for ki, kt in enumerate(kts):
                                    steps.append((t, mp, ki, kt, len(kts)))
                        bo, bd = 4, 5
                        info = {}

                        def s_exp(i):
                            t, mp, ki, kt, nk = steps[i]
                            t0, tn = TILES[t]
                            b_ = i % 4
                            S.op("pe", lambda e: e.matmul(ps[b_][:, :tn], kh[:, kt * 128:(kt + 1) * 128], qz[mp][:, t0:t0 + tn],
                                                          start=True, stop=True),
                                 reads=["kh", "qh"], writes=[psk[b_]])
                            Et, ek = E[i % 3], ("E", i % 3)
                            S.op("act", lambda e: e.activation(out=Et[:, :tn], in_=ps[b_][:, :tn], func=AF.Exp, scale=ATTN_SCALE),
                                 reads=[psk[b_]], writes=[ek])

                        deferred = {}

                        def defer(step, fn):
                            deferred.setdefault(step, []).append(fn)

                        def pv_d(i):
                            nonlocal mi
                            t, mp, ki, kt, nk = steps[i]
                            t0, tn = TILES[t]
                            Et, ek = E[i % 3], ("E", i % 3)
                            S.op("pe", lambda e: e.matmul(ps[bo][:, :tn], v[:, kt, hd * 128:(hd + 1) * 128], Et[:, :tn],
                                                          start=(ki == 0), stop=(ki == nk - 1)),
                                 reads=[ek, "v"], writes=[psk[bo]])
                            S.op("pe", lambda e: e.matmul(ps[bd][:, :tn], onesb, Et[:, :tn],
                                                          start=(ki == 0), stop=(ki == nk - 1)),
                                 reads=[ek, "const"], writes=[psk[bd]])
                            if ki != nk - 1:
                                return
                            S.op("act", lambda e: e.activation(out=rs[:, :tn], in_=ps[bd][:, :tn], func=AF.Ln), reads=[psk[bd]], writes=["rs"])
                            S.op("act", lambda e: e.activation(out=rs[:, :tn], in_=rs[:, :tn], func=AF.Exp, scale=-1.0), reads=["rs"], writes=["rs"])
                            S.op("dve", lambda e: e.tensor_tensor(rm[mp][:, :tn], ps[bo][:, :tn], rs[:, :tn], ALU.mult),
                                 reads=[psk[bo], "rs"], writes=[("rm", mp)])
                            if mp == 0:
                                return
                            S.op("dve", lambda e: e.scalar_tensor_tensor(at[:, :tn], rm[1][:, :tn], lamv[:, l, 2:3], rm[0][:, :tn], ALU.mult, ALU.add),
                                 reads=[("rm", 0), ("rm", 1), "lamv"], writes=["at"])
                            S.op("act", lambda e: e.activation(out=sqt[:, :tn], in_=at[:, :tn], func=AF.Square), reads=["at"], writes=["sqt"])
                            mx, km = mixt[mi % 2], ("amix", mi % 2)
                            mi += 1
                            n = tile_n(t, s)

                            def stage2():
                                b_ = bank("d")
                                S.op("pe", lambda e: e.matmul(ps[b_][:, :tn], ones_h, sqt[:, :tn], start=True, stop=True),
                                     reads=["sqt", "const"], writes=[psk[b_]])
                                S.op("dve", lambda e: e.tensor_scalar(sqt[:, :tn], ps[b_][:, :tn], RMS_EPS, None, ALU.add), reads=[psk[b_], "sqt"], writes=["sqt"])
                                S.op("act", lambda e: e.activation(out=sqt[:, :tn], in_=sqt[:, :tn], func=AF.Ln), reads=["sqt"], writes=["sqt"])
                                S.op("act", lambda e: e.activation(out=sqt[:, :tn], in_=sqt[:, :tn], func=AF.Exp, scale=-0.5), reads=["sqt"], writes=["sqt"])
                                S.op("dve", lambda e: e.scalar_tensor_tensor(mx[:, :tn], at[:, :tn], subg[:, l:l + 1], sqt[:, :tn], ALU.mult, ALU.mult),
                                     reads=["at", "sqt", "subg"], writes=[km])

                            def stage3(m):
                                def f():
                                    b_ = bank("d")
                                    S.op("pe", lambda e: e.matmul(ps[b_][:, :tn], wo[sl][:, m * 128:(m + 1) * 128], mx[:, :tn], start=True, stop=True),
                                         reads=[("attw", sl), km], writes=[psk[b_]])
                                    S.op("dve", lambda e: e.scalar_tensor_tensor(h[:, m, t0:t0 + tn], ps[b_][:, :tn], M(l, 16 + m, n),
                                                                                 h[:, m, t0:t0 + tn], ALU.mult, ALU.add),
                                         reads=[psk[b_], hk(m, t)], writes=[hk(m, t)])
                                return f
                            cur = i + LA
                            defer(cur + 6, stage2)
                            for m in range(8):
                                defer(cur + 13 + m, stage3(m))
                        nst = len(steps)
                        i = 0
                        while i < nst + LA or any(k >= i for k in deferred):
                            if i < nst:
                                s_exp(i)
                            if 0 <= i - LA < nst:
                                pv_d(i - LA)
                            for fn in deferred.pop(i, []):
                                fn()
                            i += 1
                    S.barrier()

                moe = (l % 2 == 1)
                jj = l // 2
                with ExitStack() as ph2:
                    lg = sb(ph2, "lg", [128, 18, NE]) if moe else None
                    comb = sb(ph2, "comb", [128, 18, NE]) if moe else None
                    with ExitStack() as ph:
                        if run("ln1"):
                            layer_norm(l, s, 1, qtiles, (l, 32, 24), ph, router_j=(jj if moe else None), lg=lg)
                        S.barrier()

                    with ExitStack() as ph:
                        w1b = [sb(ph, f"w1b{i}", [128, 8, 256], BF16) for i in range(2)]
                        w3b = [sb(ph, f"w3b{i}", [128, 8, 256], BF16) for i in range(2)]
                        w2b = [sb(ph, f"w2b{i}", [128, 2, D], BF16) for i in range(2)]
                        act_ = [sb(ph, f"act{i}", [128, 2, T], BF16) for i in range(2)]
                        st_ = [sb(ph, f"st{i}", [128, 512]) for i in range(2)]
                        evt = [sb(ph, f"evt{i}", [128, 512]) for i in range(2)]
                        dj = 0
                        ej = 0
                        dvr = 0
                        pt_ = [sb(ph, f"pt{i}", [128, 512]) for i in range(2)] if moe else None
                        cbt = [sb(ph, f"cbt{i}", [128, T]) for i in range(2)] if moe else None
                        dg = [sb(ph, f"dg{i}", [128, 128]) for i in range(2)] if moe else None
                        sm = sb(ph, "sm", [128, 8]) if moe else None
                        if moe and run("ffn"):
                            for tt in (range(18) if not last else range(2, 18)):
                                L = lg[:, tt, :]
                                Cb = comb[:, tt, :]
                                S.op("dve", lambda e: e.reduce_max(sm[:, 0:1], L, AX.X), reads=["lg", "sm"], writes=["sm"])
                                S.op("dve", lambda e: e.tensor_scalar(Cb, L, sm[:, 0:1], None, ALU.is_equal), reads=["lg", "sm"], writes=["comb"])
                                S.op("dve", lambda e: e.scalar_tensor_tensor(L, Cb, -1e30, L, ALU.mult, ALU.add), reads=["comb", "lg"], writes=["lg"])
                                S.op("dve", lambda e: e.reduce_max(sm[:, 1:2], L, AX.X), reads=["lg", "sm"], writes=["sm"])
                                S.op("dve", lambda e: e.tensor_scalar(L, L, sm[:, 1:2], None, ALU.is_equal), reads=["lg", "sm"], writes=["lg"])
                                S.op("dve", lambda e: e.tensor_tensor(sm[:, 2:3], sm[:, 1:2], sm[:, 0:1], ALU.subtract), reads=["sm"], writes=["sm"])
                                S.op("act", lambda e: e.activation(out=sm[:, 3:4], in_=sm[:, 2:3], func=AF.Exp), reads=["sm"], writes=["sm"])
                                S.op("dve", lambda e: e.tensor_scalar(sm[:, 4:5], sm[:, 3:4], 1.0, None, ALU.add), reads=["sm"], writes=["sm"])
                                S.op("dve", lambda e: e.reciprocal(sm[:, 5:6], sm[:, 4:5]), reads=["sm"], writes=["sm"])
                                S.op("dve", lambda e: e.tensor_tensor(sm[:, 6:7], sm[:, 3:4], sm[:, 5:6], ALU.mult), reads=["sm"], writes=["sm"])
                                S.op("dve", lambda e: e.tensor_scalar(Cb, Cb, sm[:, 5:6], None, ALU.mult), reads=["comb", "sm"], writes=["comb"])
                                S.op("dve", lambda e: e.scalar_tensor_tensor(Cb, L, sm[:, 6:7], Cb, ALU.mult, ALU.add), reads=["comb", "lg", "sm"], writes=["comb"])
                        experts = range(NE) if moe else range(1)
                        gi = 0

                        def wsrc(e_):
                            if moe:
                                return moe_w1[jj, e_], moe_w3[jj, e_], moe_w2[jj, e_]
                            return ffn_w1[jj], ffn_w3[jj], ffn_w2[jj]

                        def load_fg(e_, g, sl):
                            a1, a3, a2 = wsrc(e_)
                            a1v = a1.rearrange("(c p) f -> p c f", p=128)
                            a3v = a3.rearrange("(c p) f -> p c f", p=128)
                            a2v = a2.rearrange("(c p) d -> p c d", p=128)
                            S.dma("pool", f"ffw{sl}", [(w1b[sl][:], a1v[:, :, g * 256:(g + 1) * 256]),
                                                       (w3b[sl][:], a3v[:, :, g * 256:(g + 1) * 256]),
                                                       (w2b[sl][:], a2v[:, 2 * g:2 * g + 2, :])],
                                  writes=[("ffw", sl)])
                        seq_fg = [(e_, g) for e_ in experts for g in range(NFG)] if run("ffn") else []
                        if seq_fg:
                            load_fg(seq_fg[0][0], seq_fg[0][1], 0)
                        si = 0
                        for idx, (e_, g) in enumerate(seq_fg):
                            sl = idx % 2
                            if idx + 1 < len(seq_fg):
                                load_fg(seq_fg[idx + 1][0], seq_fg[idx + 1][1], (idx + 1) % 2)
                            if moe and g == 0:
                                cbe = cbt[e_ % 2]
                                for t in qtiles:
                                    t0, tn = TILES[t]
                                    b_ = bank("d")
                                    for q in range(tn // 128):
                                        tt = t0 // 128 + q
                                        d_ = dg[tt % 2]
                                        S.op("dve", lambda e: e.tensor_scalar(d_[:], ident, comb[:, tt, e_:e_ + 1], None, ALU.mult),
                                             reads=["comb", "const"], writes=[("dg", tt % 2)])
                                        S.op("pe", lambda e: e.matmul(ps[b_][:, q * 128:(q + 1) * 128], ones_1, d_[:], start=True, stop=True),
                                             reads=[("dg", tt % 2), "const"], writes=[psk[b_]])
                                    S.op("act", lambda e: e.activation(out=cbe[:, t0:t0 + tn], in_=ps[b_][:, :tn], func=AF.Identity), reads=[psk[b_]], writes=[("cbt", e_ % 2)])
                            ab = act_[sl]
                            for t in qtiles:
                                t0, tn = TILES[t]
                                for f in range(2):
                                    b1, b3 = bank("a"), bank("b")
                                    for k in range(8):
                                        S.op("pe", lambda e: e.matmul(ps[b1][:, :tn], w1b[sl][:, k, f * 128:(f + 1) * 128], u[:, k, t0:t0 + tn],
                                                                      start=(k == 0), stop=(k == 7)),
                                             reads=[("ffw", sl), uk(t)], writes=[psk[b1]])
                                    for k in range(8):
                                        S.op("pe", lambda e: e.matmul(ps[b3][:, :tn], w3b[sl][:, k, f * 128:(f + 1) * 128], u[:, k, t0:t0 + tn],
                                                                      start=(k == 0), stop=(k == 7)),
                                             reads=[("ffw", sl), uk(t)], writes=[psk[b3]])
                                    stt, sk = st_[si % 2], ("st", si % 2)
                                    S.op("act", lambda e: e.activation(out=stt[:, :tn], in_=ps[b1][:, :tn], func=AF.Silu), reads=[psk[b1]], writes=[sk])
                                    if moe:
                                        ptt, pk = pt_[si % 2], ("pt", si % 2)
                                        S.op("dve", lambda e: e.tensor_tensor(ptt[:, :tn], stt[:, :tn], ps[b3][:, :tn], ALU.mult),
                                             reads=[sk, psk[b3]], writes=[pk])
                                        S.op("dve", lambda e: e.tensor_tensor(ab[:, f, t0:t0 + tn], ptt[:, :tn], cbt[e_ % 2][:, t0:t0 + tn], ALU.mult),
                                             reads=[pk, ("cbt", e_ % 2)], writes=[("act", sl, t, f)])
                                    else:
                                        S.op("dve", lambda e: e.tensor_tensor(ab[:, f, t0:t0 + tn], stt[:, :tn], ps[b3][:, :tn], ALU.mult),
                                             reads=[sk, psk[b3]], writes=[("act", sl, t, f)])
                                    si += 1
                            for m in range(8):
                                for t in qtiles:
                                    t0, tn = TILES[t]
                                    n = tile_n(t, s)
                                    via_act = (dj % 8) in (1, 4, 6)
                                    dj += 1
                                    if via_act:
                                        b_ = bank("d")
                                    else:
                                        b_ = (4, 5, 0, 1, 2, 3)[dvr % 6]
                                        dvr += 1
                                    for f in range(2):
                                        S.op("pe", lambda e: e.matmul(ps[b_][:, :tn], w2b[sl][:, f, m * 128:(m + 1) * 128], ab[:, f, t0:t0 + tn],
                                                                      start=(f == 0), stop=(f == 1)),
                                             reads=[("ffw", sl), ("act", sl, t, f)], writes=[psk[b_]])
                                    if via_act:
                                        ev_, evk = evt[ej % 2], ("evt", ej % 2)
                                        ej += 1
                                        S.op("act", lambda e: e.activation(out=ev_[:, :tn], in_=ps[b_][:, :tn], func=AF.Identity, scale=M(l, 40 + m, n)),
                                             reads=[psk[b_], "mod"], writes=[evk])
                                        S.op("pool", lambda e: e.tensor_tensor(h[:, m, t0:t0 + tn], h[:, m, t0:t0 + tn], ev_[:, :tn], ALU.add),
                                             reads=[evk, hk(m, t)], writes=[hk(m, t)])
                                    else:
                                        S.op("dve", lambda e: e.scalar_tensor_tensor(h[:, m, t0:t0 + tn], ps[b_][:, :tn], M(l, 40 + m, n),
                                                                                     h[:, m, t0:t0 + tn], ALU.mult, ALU.add),
                                             reads=[psk[b_], hk(m, t)], writes=[hk(m, t)])
                        S.barrier()

                with ExitStack() as ph:
                    nl = (l + 1 < NL)
                    if run("ln2"):
                        layer_norm(l, s, 2, qtiles, ((l + 1, 8, 0) if nl else None), ph)
                    S.barrier()

            yv = yT[s].rearrange("(c p) t -> p c t", p=128)
            S.dma("sp", "yst", [(yv[:, 0:4, :], h[:, 0:4, CTX:]), (yv[:, 4:8, :], h[:, 4:8, CTX:])],
                  reads=[hk(c, t) for c in range(8) for t in range(5)], writes=["y"])
            S.final_wait("sp", "yst")
            S.barrier()
    return nc


def _rope_tables():
    rows = SEQ // 64
    row = np.repeat(np.arange(rows, dtype=np.float32), 64)
    col = np.tile(np.arange(64, dtype=np.float32), rows)
    inv = (10000.0 ** (-np.arange(16, dtype=np.float32) / 16)).astype(np.float32)
    ang = np.concatenate([row[:, None] * inv, col[:, None] * inv], -1)
    jj = (np.arange(128) % 64) // 2
    return np.ascontiguousarray(np.cos(ang)[:, jj].T.astype(np.float32)), np.ascontiguousarray(np.sin(ang)[:, jj].T.astype(np.float32))


def _host_layout(inp):
    f = lambda a: np.ascontiguousarray(np.asarray(a, dtype=np.float32))
    sh = {}
    sh["w_mod"] = f(inp["w_mod"])
    sh["b_modT"] = f(np.asarray(inp["b_mod"]).reshape(DEPTH, 48, 128).transpose(2, 0, 1))
    sh["w_in"] = f(inp["w_in"])
    sh["w_out"] = f(inp["w_out"])
    lam = np.stack([np.asarray(inp[k]) for k in ("lam_q1", "lam_k1", "lam_q2", "lam_k2")], 1)
    sh["lamqk"] = f(np.broadcast_to(lam[None], (128, DEPTH, 4, 64)))
    sh["sublnT"] = f(np.asarray(inp["subln_g"]).T)
    cw = np.asarray(inp["conv_w"]).reshape(DEPTH, 4, 4, 128)
    cbias = np.asarray(inp["conv_b"]).reshape(DEPTH, 1, 4, 128)
    sh["convT"] = f(np.concatenate([cw, cbias], 1).transpose(3, 0, 2, 1))
    rv = np.stack([np.asarray(inp[k]).reshape(DEPTH, 2, 4, 128) for k in ("rg_ba", "rg_bx", "rg_lambda")], -1)
    sh["rgvT"] = f(rv.transpose(3, 0, 1, 2, 4))
    gb = np.zeros((DEPTH, 2, 4, 2, 128, 128), np.float32)
    for g, k in enumerate(("rg_wa", "rg_wx")):
        w = np.asarray(inp[k])
        for j in range(4):
            gb[:, :, j, g, 0:64, 0:64] = w[:, :, 2 * j]
            gb[:, :, j, g, 64:128, 64:128] = w[:, :, 2 * j + 1]
    sh["gate_bd"] = gb
    ln = np.stack([np.asarray(inp[k]).reshape(DEPTH, 8, 128) for k in ("ln1_g", "ln1_b", "ln2_g", "ln2_b")], 1)
    sh["lnT"] = f(ln.transpose(3, 0, 1, 2))
    for k in ("ffn_w1", "ffn_w3", "ffn_w2", "moe_w1", "moe_w3", "moe_w2"):
        sh[k] = f(inp[k])
    sh["routerT"] = f(np.asarray(inp["moe_router"]).reshape(2, 8, 128, NE).transpose(2, 0, 1, 3))
    c, s_ = _rope_tables()
    sh["cosd"], sh["sind"] = c, s_
    cfm = np.zeros((128, 4, 128), np.float32)
    cfm[:, 0, :] = np.eye(128, dtype=np.float32)
    cfm[:, 1, :] = 1.0 / 1024
    cfm[:, 2, :] = 1.0 / 128
    cfm[:, 3, :] = 1.0
    sh["constf"] = cfm
    cbm = np.zeros((128, 2, 128), np.float32)
    cbm[:, 0, :] = 1.0
    for j in range(64):
        cbm[2 * j + 1, 1, 2 * j] = -1.0
        cbm[2 * j, 1, 2 * j + 1] = 1.0
    sh["constb"] = cbm.astype(ml_dtypes.bfloat16)
    return sh


def kernel(**inputs):
    NL, NSEQ, NC = CFG["NL"], CFG["NSEQ"], CFG["NCORES"]
    shared = _host_layout(inputs)
    x = np.asarray(inputs["x"], dtype=np.float32)
    ctx = np.asarray(inputs["ctx"], dtype=np.float32)
    c = np.asarray(inputs["c"], dtype=np.float32)
    c_ctx = np.asarray(inputs["c_ctx"], dtype=np.float32)
    in_maps = []
    for core in range(NC):
        bs = [core * NSEQ + i for i in range(NSEQ)]
        m = dict(shared)
        m["xT"] = np.ascontiguousarray(x[bs].transpose(0, 2, 1))
        m["ctxT"] = np.ascontiguousarray(ctx[bs].transpose(0, 2, 1))
        cols = [c[b] for b in bs]
        while len(cols) < 2:
            cols.append(cols[-1])
        cols.append(c_ctx)
        m["cT"] = np.ascontiguousarray(np.stack(cols, -1).reshape(8, 128, 3).transpose(1, 0, 2))
        in_maps.append(m)
    nc = build(NL, NSEQ)
    res = run_bass_kernel_spmd(nc, in_maps, core_ids=list(range(NC)))
    out = np.empty((NC * NSEQ, SEQ, D), np.float32)
    for core in range(NC):
        y = res.results[core]["yT"]
        for i in range(NSEQ):
            out[core * NSEQ + i] = y[i].T
    return out
```
